# Optimizing a Trainium2 kernel written in Bass

```python
import math
import jax, jax.numpy as jnp
from jax import lax
import numpy as np

D_MODEL = 2048
BATCH = 4
SEQ = 2048
DEPTH = 2
DEC_BATCH = 8
DEC_SEQ = 1
PAST_LEN = 16384
PAGE_SIZE = 128

FOX_HEADS = 4
FOX_HEAD_DIM = 128
FOX_WIDTH = FOX_HEADS * FOX_HEAD_DIM
FOX_QBLOCK = 128
FOX_F_BIASES = (2.0, 4.0, 6.0, 8.0)
POOL_GROUPS = 4
POOL_GROUP_DIM = 128
POOL_WIDTH = POOL_GROUPS * POOL_GROUP_DIM
POOL_WINDOWS = (2, 4, 8, 16)
POOL_HIST = 15
RET_HEADS = 4
RET_DK = 128
RET_DV = 128
RET_CHUNK = 128
ROPE_BASE = 10000.0
GLA_HEADS = 4
GLA_DK = 64
GLA_DV = 128
GLA_RANK = 16
GLA_TAU = 16.0
GLA_CHUNK = 64
N_BRANCH = 4
BRANCH_WIDTH = 512
D_FF = 5632
N_EXPERTS = 8
TOP_K = 2
D_FF_EXPERT = 2816
N_DENSE = (DEPTH + 1) // 2
N_MOE = DEPTH // 2
DEEPNORM_ALPHA = (2.0 * DEPTH) ** 0.25
DEEPNORM_BETA = (8.0 * DEPTH) ** -0.25
LN_EPS = 1e-5
POOL_FACTOR = 1.25

IN_SPLITS = (FOX_WIDTH, FOX_WIDTH, FOX_WIDTH, FOX_HEADS,
             POOL_WIDTH,
             RET_HEADS * RET_DK, RET_HEADS * RET_DK, RET_HEADS * RET_DV, RET_HEADS * RET_DV,
             GLA_HEADS * GLA_DK, GLA_HEADS * GLA_DK, GLA_HEADS * GLA_DV, GLA_HEADS * GLA_DV, GLA_RANK,
             N_BRANCH * D_MODEL)
D_IN = sum(IN_SPLITS)

kernel_name = 'hybrid_fox_pool_ret_gla_deepnorm_adaln_step'


def _chunk_len(length, chunk):
    return chunk if length % chunk == 0 else length


def _split_cols(z):
    idx = np.cumsum(IN_SPLITS)[:-1].tolist()
    return jnp.split(z, idx, axis=-1)


def _layernorm(x, g, b):
    xf = x.astype(jnp.float32)
    mu = jnp.mean(xf, axis=-1, keepdims=True)
    var = jnp.mean(jnp.square(xf - mu), axis=-1, keepdims=True)
    return ((xf - mu) * lax.rsqrt(var + LN_EPS) * g + b).astype(x.dtype)


def _groupnorm_heads(o):
    mu = jnp.mean(o, axis=-1, keepdims=True)
    var = jnp.mean(jnp.square(o - mu), axis=-1, keepdims=True)
    return (o - mu) * lax.rsqrt(var + LN_EPS)


def _rmsnorm_heads(o):
    return o * lax.rsqrt(jnp.mean(jnp.square(o), axis=-1, keepdims=True) + LN_EPS)


def _rotary(x, pos):
    half = x.shape[-1] // 2
    freqs = ROPE_BASE ** (-jnp.arange(half, dtype=jnp.float32) / half)
    ang = pos.astype(jnp.float32)[:, None] * freqs[None, :]
    cos = jnp.cos(ang)[None, :, None, :]
    sin = jnp.sin(ang)[None, :, None, :]
    xf = x.astype(jnp.float32)
    x1, x2 = xf[..., :half], xf[..., half:]
    return jnp.concatenate([x1 * cos - x2 * sin, x2 * cos + x1 * sin], axis=-1)


def _fox_attend(q, k, v, f_q, f_k, q_start):
    B, Lq, H, dh = q.shape
    Lk = k.shape[1]
    blk = _chunk_len(Lq, FOX_QBLOCK)
    nb = Lq // blk
    kf = k.astype(jnp.float32)
    vf = v.astype(jnp.float32)
    fk_t = f_k.astype(jnp.float32).transpose(0, 2, 1)
    kpos = jnp.arange(Lk)
    qb = (q.astype(jnp.float32) * dh ** -0.5).reshape(B, nb, blk, H, dh).swapaxes(0, 1)
    fqb = f_q.astype(jnp.float32).reshape(B, nb, blk, H).swapaxes(0, 1)

    def one_block(args):
        i, qi, fqi = args
        qpos = q_start + i * blk + jnp.arange(blk)
        s = jnp.einsum('bqhd,bkhd->bhqk', qi, kf)
        s = s + fqi.transpose(0, 2, 1)[..., None] - fk_t[:, :, None, :]
        s = jnp.where((kpos[None, :] <= qpos[:, None])[None, None], s, -jnp.inf)
        p = jax.nn.softmax(s, axis=-1)
        return jnp.einsum('bhqk,bkhd->bqhd', p, vf)

    o = lax.map(one_block, (jnp.arange(nb), qb, fqb))
    return o.swapaxes(0, 1).reshape(B, Lq, H * dh)


def _pool_mix(p, hist, pos0, w_map, scale):
    B, L, _ = p.shape
    ext_raw = jnp.concatenate([hist.astype(p.dtype), p], axis=1)
    ext = ext_raw.astype(jnp.float32)
    csum = jnp.concatenate([jnp.zeros((B, 1, POOL_WIDTH), jnp.float32), jnp.cumsum(ext, axis=1)], axis=1)
    pos = pos0 + jnp.arange(L)
    end = POOL_HIST + 1
    means = []
    for g, w in enumerate(POOL_WINDOWS):
        cs = slice(g * POOL_GROUP_DIM, (g + 1) * POOL_GROUP_DIM)
        wsum = csum[:, end:end + L, cs] - csum[:, end - w:end - w + L, cs]
        cnt = jnp.minimum(w, pos + 1).astype(jnp.float32)[None, :, None]
        means.append(wsum / cnt)
    mean = jnp.stack(means, axis=2)
    pg = p.astype(jnp.float32).reshape(B, L, POOL_GROUPS, POOL_GROUP_DIM)
    y = jnp.einsum('blgc,gcd->blgd', mean - pg, w_map.astype(jnp.float32)).reshape(B, L, POOL_WIDTH)
    return y * scale.astype(jnp.float32), ext_raw[:, -POOL_HIST:]


def _retention(q, k, v, s0):
    B, L, H, _ = q.shape
    C = _chunk_len(L, RET_CHUNK)
    nc = L // C
    logg = jnp.log1p(-jnp.exp2(-5.0 - jnp.arange(H, dtype=jnp.float32)))
    n = jnp.arange(C, dtype=jnp.float32)
    diff = n[:, None] - n[None, :]
    causal = diff >= 0
    dmat = jnp.where(causal[None], jnp.exp(jnp.where(causal, diff, 0.0)[None] * logg[:, None, None]), 0.0)
    xi = jnp.exp((n[None, :] + 1.0) * logg[:, None]).T
    zeta = jnp.exp((C - 1.0 - n)[None, :] * logg[:, None]).T
    g_c = jnp.exp(C * logg)

    def to_chunks(a):
        return a.reshape(B, nc, C, *a.shape[2:]).swapaxes(0, 1)

    def step(s, inp):
        qc, kc, vc = inp
        att = jnp.einsum('bnhk,bmhk->bhnm', qc, kc) * dmat[None]
        o = jnp.einsum('bhnm,bmhv->bnhv', att, vc)
        o = o + jnp.einsum('bnhk,bhkv->bnhv', qc * xi[None, :, :, None], s)
        s = g_c[None, :, None, None] * s + jnp.einsum('bmhk,bmhv->bhkv', kc * zeta[None, :, :, None], vc)
        return s, o

    s, o = lax.scan(step, s0, (to_chunks(q), to_chunks(k), to_chunks(v)))
    return o.swapaxes(0, 1).reshape(B, L, H, v.shape[-1]), s


def _gla(q, k, v, loga, s0):
    B, L, H, _ = q.shape
    C = _chunk_len(L, GLA_CHUNK)
    nc = L // C
    causal = jnp.tril(jnp.ones((C, C), dtype=bool))[None, :, :, None, None]

    def to_chunks(a):
        return a.reshape(B, nc, C, *a.shape[2:]).swapaxes(0, 1)

    def step(s, inp):
        qc, kc, vc, ac = inp
        b = jnp.cumsum(ac, axis=1)
        o = jnp.einsum('bthk,bhkv->bthv', qc * jnp.exp(b), s)
        rel = jnp.exp(jnp.where(causal, b[:, :, None] - b[:, None, :], -jnp.inf))
        att = jnp.einsum('btshk,bshk->bhts', qc[:, :, None] * rel, kc)
        o = o + jnp.einsum('bhts,bshv->bthv', att, vc)
        b_last = b[:, -1]
        s = jnp.exp(b_last)[..., None] * s + jnp.einsum('bshk,bshv->bhkv', kc * jnp.exp(b_last[:, None] - b), vc)
        return s, o

    s, o = lax.scan(step, s0, (to_chunks(q), to_chunks(k), to_chunks(v), to_chunks(loga)))
    return o.swapaxes(0, 1).reshape(B, L, H, v.shape[-1]), s


def _mixer(u, past, w_in_l, b_f_l, w_a2_l, b_a_l, w_map_l, pscale_l, w_branch_l, w_o_l):
    k_past, v_past, logf_past, hist, s_ret, s_gla = past
    B, L, _ = u.shape
    P = k_past.shape[1]
    f32 = jnp.float32
    pos = P + jnp.arange(L)
    z = jnp.einsum('bld,de->ble', u, w_in_l)
    (fq, fk, fv, ff, pin, rq, rk, rv, rg, gq, gk, gv, gg, glr, gates) = _split_cols(z)

    q = fq.reshape(B, L, FOX_HEADS, FOX_HEAD_DIM)
    k = fk.reshape(B, L, FOX_HEADS, FOX_HEAD_DIM)
    v = fv.reshape(B, L, FOX_HEADS, FOX_HEAD_DIM)
    logf = jax.nn.log_sigmoid(ff.astype(f32) + b_f_l.astype(f32))
    k_all = jnp.concatenate([k_past.astype(k.dtype), k], axis=1)
    v_all = jnp.concatenate([v_past.astype(v.dtype), v], axis=1)
    logf_all = jnp.concatenate([logf_past.astype(f32), logf], axis=1)
    f_cum = logf_all - lax.cumsum(logf_all, axis=1, reverse=True)
    y_fox = _fox_attend(q, k_all, v_all, f_cum[:, P:], f_cum, P)

    y_pool, new_hist = _pool_mix(pin, hist, P, w_map_l, pscale_l)

    qr = _rotary(rq.reshape(B, L, RET_HEADS, RET_DK), pos) * RET_DK ** -0.5
    kr = _rotary(rk.reshape(B, L, RET_HEADS, RET_DK), pos)
    o_r, s_ret_new = _retention(qr, kr, rv.reshape(B, L, RET_HEADS, RET_DV).astype(f32), s_ret.astype(f32))
    y_ret = (_groupnorm_heads(o_r) * jax.nn.silu(rg.astype(f32)).reshape(B, L, RET_HEADS, RET_DV)).reshape(B, L, -1)

    loga = jax.nn.log_sigmoid(jnp.einsum('blr,rk->blk', glr, w_a2_l).astype(f32) + b_a_l.astype(f32)) / GLA_TAU
    o_g, s_gla_new = _gla(gq.reshape(B, L, GLA_HEADS, GLA_DK).astype(f32) * GLA_DK ** -0.5,
                          gk.reshape(B, L, GLA_HEADS, GLA_DK).astype(f32),
                          gv.reshape(B, L, GLA_HEADS, GLA_DV).astype(f32),
                          loga.reshape(B, L, GLA_HEADS, GLA_DK),
                          s_gla.astype(f32))
    y_gla = (_rmsnorm_heads(o_g) * jax.nn.silu(gg.astype(f32)).reshape(B, L, GLA_HEADS, GLA_DV)).reshape(B, L, -1)

    Y = jnp.stack([y_fox, y_pool, y_ret, y_gla], axis=2).astype(u.dtype)
    ybr = jnp.einsum('blnc,ncd->blnd', Y, w_branch_l).astype(f32)
    gate = jax.nn.sigmoid(gates.reshape(B, L, N_BRANCH, D_MODEL).astype(f32))
    merged = jnp.sum(gate * ybr, axis=2).astype(u.dtype)
    out = jnp.einsum('bld,de->ble', merged, w_o_l)
    return out, (k, v, logf, new_hist, s_ret_new, s_gla_new)


def _swiglu(v, wg, wu, wd):
    h = jax.nn.silu(jnp.einsum('bld,df->blf', v, wg)) * jnp.einsum('bld,df->blf', v, wu)
    return jnp.einsum('blf,fd->bld', h, wd)


def _moe(v, w_r, b_r, wg, wu, wd):
    logits = jnp.einsum('bld,de->ble', v, w_r).astype(jnp.float32) + b_r.astype(jnp.float32)
    top_val, top_idx = lax.top_k(logits, TOP_K)
    top_w = jax.nn.softmax(top_val, axis=-1)
    gates = jnp.sum(jax.nn.one_hot(top_idx, N_EXPERTS, dtype=jnp.float32) * top_w[..., None], axis=-2)
    out = jnp.zeros(v.shape, jnp.float32)
    for e in range(N_EXPERTS):
        out = out + gates[..., e:e + 1] * _swiglu(v, wg[e], wu[e], wd[e]).astype(jnp.float32)
    return out.astype(v.dtype)


def _block(x, c, w_ada_l, b_ada_l, past, mix_w, norm_w, is_moe, ffn_w):
    mod = jnp.dot(jax.nn.silu(c), w_ada_l) + b_ada_l
    sh1, sc1, g1, sh2, sc2, g2 = [m[:, None, :] for m in jnp.split(mod, 6, axis=-1)]
    u = x * (1 + sc1) + sh1
    y, states = _mixer(u, past, *mix_w)
    x = _layernorm(DEEPNORM_ALPHA * x + g1 * y, norm_w[0], norm_w[1])
    hv = x * (1 + sc2) + sh2
    f = _moe(hv, *ffn_w) if is_moe else _swiglu(hv, *ffn_w)
    x = _layernorm(DEEPNORM_ALPHA * x + g2 * f, norm_w[2], norm_w[3])
    return x, states


def setup_inputs(seed: int = 0) -> dict:
    key = jax.random.key(seed)
    keys = jax.random.split(key, 33)
    counter = [0]
    f32 = jnp.float32

    def nrm(shape, scale=1.0):
        kk = keys[counter[0]]
        counter[0] += 1
        return jax.random.normal(kk, shape, f32) * scale

    n_pages = PAST_LEN // PAGE_SIZE
    n_pool = int(math.ceil(POOL_FACTOR * DEC_BATCH * n_pages))
    f_bias = jnp.array(FOX_F_BIASES, f32)
    inp = {}
    inp['x_prompt'] = nrm((BATCH, SEQ, D_MODEL))
    inp['x_sample'] = nrm((DEC_BATCH, DEC_SEQ, D_MODEL))
    inp['cache_k'] = nrm((n_pool, DEPTH, PAGE_SIZE, FOX_HEADS, FOX_HEAD_DIM))
    inp['cache_v'] = nrm((n_pool, DEPTH, PAGE_SIZE, FOX_HEADS, FOX_HEAD_DIM))
    inp['cache_logf'] = jax.nn.log_sigmoid(f_bias + nrm((n_pool, DEPTH, PAGE_SIZE, FOX_HEADS)))
    inp['state_pool'] = nrm((DEC_BATCH, DEPTH, POOL_HIST, POOL_WIDTH))
    inp['state_ret'] = nrm((DEC_BATCH, DEPTH, RET_HEADS, RET_DK, RET_DV), 4.0)
    inp['state_gla'] = nrm((DEC_BATCH, DEPTH, GLA_HEADS, GLA_DK, GLA_DV), 2.0)
    kk = keys[counter[0]]
    counter[0] += 1
    inp['page_table'] = jax.random.permutation(kk, n_pool)[:DEC_BATCH * n_pages].reshape(DEC_BATCH, n_pages).astype(jnp.int32)
    inp['c_prompt'] = nrm((BATCH, D_MODEL))
    inp['c_sample'] = nrm((DEC_BATCH, D_MODEL))
    inp['w_ada'] = nrm((DEPTH, D_MODEL, 6 * D_MODEL), 0.1 * D_MODEL ** -0.5)
    gate_offset = jnp.repeat(jnp.array([0.0, 0.0, 1.0, 0.0, 0.0, 1.0], f32), D_MODEL)
    inp['b_ada'] = nrm((DEPTH, 6 * D_MODEL), 0.02) + gate_offset[None, :]
    inp['w_in'] = nrm((DEPTH, D_MODEL, D_IN), D_MODEL ** -0.5)
    inp['b_fox_f'] = f_bias[None, :] + nrm((DEPTH, FOX_HEADS), 0.1)
    inp['w_gla_a2'] = nrm((DEPTH, GLA_RANK, GLA_HEADS * GLA_DK), GLA_RANK ** -0.5)
    inp['b_gla_a'] = nrm((DEPTH, GLA_HEADS * GLA_DK), 0.1)
    inp['w_pool_map'] = nrm((DEPTH, POOL_GROUPS, POOL_GROUP_DIM, POOL_GROUP_DIM), POOL_GROUP_DIM ** -0.5)
    inp['pool_scale'] = 1.0 + nrm((DEPTH, POOL_WIDTH), 0.02)
    inp['w_branch'] = nrm((DEPTH, N_BRANCH, BRANCH_WIDTH, D_MODEL), BRANCH_WIDTH ** -0.5)
    inp['w_o'] = nrm((DEPTH, D_MODEL, D_MODEL), DEEPNORM_BETA * D_MODEL ** -0.5)
    inp['ln1_g'] = 1.0 + nrm((DEPTH, D_MODEL), 0.02)
    inp['ln1_b'] = nrm((DEPTH, D_MODEL), 0.02)
    inp['ln2_g'] = 1.0 + nrm((DEPTH, D_MODEL), 0.02)
    inp['ln2_b'] = nrm((DEPTH, D_MODEL), 0.02)
    inp['w_ff_gate'] = nrm((N_DENSE, D_MODEL, D_FF), D_MODEL ** -0.5)
    inp['w_ff_up'] = nrm((N_DENSE, D_MODEL, D_FF), D_MODEL ** -0.5)
    inp['w_ff_down'] = nrm((N_DENSE, D_FF, D_MODEL), DEEPNORM_BETA * D_FF ** -0.5)
    inp['w_router'] = nrm((N_MOE, D_MODEL, N_EXPERTS), D_MODEL ** -0.5)
    inp['b_router'] = nrm((N_MOE, N_EXPERTS), 0.01)
    inp['w_ex_gate'] = nrm((N_MOE, N_EXPERTS, D_MODEL, D_FF_EXPERT), D_MODEL ** -0.5)
    inp['w_ex_up'] = nrm((N_MOE, N_EXPERTS, D_MODEL, D_FF_EXPERT), D_MODEL ** -0.5)
    inp['w_ex_down'] = nrm((N_MOE, N_EXPERTS, D_FF_EXPERT, D_MODEL), DEEPNORM_BETA * D_FF_EXPERT ** -0.5)
    return inp


def reference(x_prompt, x_sample, cache_k, cache_v, cache_logf, state_pool, state_ret, state_gla, page_table,
              c_prompt, c_sample, w_ada, b_ada, w_in, b_fox_f, w_gla_a2, b_gla_a, w_pool_map, pool_scale,
              w_branch, w_o, ln1_g, ln1_b, ln2_g, ln2_b, w_ff_gate, w_ff_up, w_ff_down,
              w_router, b_router, w_ex_gate, w_ex_up, w_ex_down):
    f32 = jnp.float32
    hp, hs = x_prompt, x_sample
    nb_p = hp.shape[0]
    nb_s = hs.shape[0]
    new_p = [[] for _ in range(6)]
    new_s = [[] for _ in range(6)]
    for l in range(DEPTH):
        mix_w = (w_in[l], b_fox_f[l], w_gla_a2[l], b_gla_a[l], w_pool_map[l], pool_scale[l], w_branch[l], w_o[l])
        norm_w = (ln1_g[l], ln1_b[l], ln2_g[l], ln2_b[l])
        is_moe = (l % 2) == 1
        j = l // 2
        if is_moe:
            ffn_w = (w_router[j], b_router[j], w_ex_gate[j], w_ex_up[j], w_ex_down[j])
        else:
            ffn_w = (w_ff_gate[j], w_ff_up[j], w_ff_down[j])
        past_p = (jnp.zeros((nb_p, 0, FOX_HEADS, FOX_HEAD_DIM), hp.dtype),
                  jnp.zeros((nb_p, 0, FOX_HEADS, FOX_HEAD_DIM), hp.dtype),
                  jnp.zeros((nb_p, 0, FOX_HEADS), f32),
                  jnp.zeros((nb_p, POOL_HIST, POOL_WIDTH), hp.dtype),
                  jnp.zeros((nb_p, RET_HEADS, RET_DK, RET_DV), f32),
                  jnp.zeros((nb_p, GLA_HEADS, GLA_DK, GLA_DV), f32))
        hp, st_p = _block(hp, c_prompt, w_ada[l], b_ada[l], past_p, mix_w, norm_w, is_moe, ffn_w)
        for lst, a in zip(new_p, st_p):
            lst.append(a)
        k_past = cache_k[page_table, l].reshape(nb_s, -1, FOX_HEADS, FOX_HEAD_DIM)
        v_past = cache_v[page_table, l].reshape(nb_s, -1, FOX_HEADS, FOX_HEAD_DIM)
        logf_past = cache_logf[page_table, l].reshape(nb_s, -1, FOX_HEADS)
        past_s = (k_past, v_past, logf_past, state_pool[:, l], state_ret[:, l], state_gla[:, l])
        hs, st_s = _block(hs, c_sample, w_ada[l], b_ada[l], past_s, mix_w, norm_w, is_moe, ffn_w)
        for lst, a in zip(new_s, st_s):
            lst.append(a)
    new_k_prompt = jnp.stack(new_p[0], axis=1)
    new_v_prompt = jnp.stack(new_p[1], axis=1)
    new_logf_prompt = jnp.stack(new_p[2], axis=1)
    new_pool_prompt = jnp.stack(new_p[3], axis=1)
    new_ret_prompt = jnp.stack(new_p[4], axis=1)
    new_gla_prompt = jnp.stack(new_p[5], axis=1)
    new_k_sample = jnp.stack(new_s[0], axis=1)
    new_v_sample = jnp.stack(new_s[1], axis=1)
    new_logf_sample = jnp.stack(new_s[2], axis=1)
    new_pool_sample = jnp.stack(new_s[3], axis=1)
    new_ret_sample = jnp.stack(new_s[4], axis=1)
    new_gla_sample = jnp.stack(new_s[5], axis=1)
    return (hp, hs, new_k_prompt, new_v_prompt, new_logf_prompt, new_pool_prompt, new_ret_prompt, new_gla_prompt,
            new_k_sample, new_v_sample, new_logf_sample, new_pool_sample, new_ret_sample, new_gla_sample)
```

```python
import math
from contextlib import ExitStack

import numpy as np
import concourse.bass as bass
import concourse.mybir as mybir
from concourse.bass_utils import run_bass_kernel_spmd

F32 = mybir.dt.float32
BF16 = mybir.dt.bfloat16
I32 = mybir.dt.int32
AF = mybir.ActivationFunctionType
ALU = mybir.AluOpType
AX = mybir.AxisListType

D = 2048
KC = 16
T = 2048
NS = 2
TT = T + NS
NCORE = 4
DEPTH = 2
PAST = 16384
NPAGE = 128
N_POOL = 1280
D_IN = 13844
OFF = dict(fq=0, fk=512, fv=1024, ff=1536, pin=1540, rq=2052, rk=2564, rv=3076, rg=3588,
           gq=4100, gk=4356, gv=4612, gg=5124, glr=5636, gates=5652)
NB = 5
BW = 410
ALPHA = (2.0 * DEPTH) ** 0.25
LN_EPS = 1e-5
COLT = [(0, 512), (512, 512), (1024, 512), (1536, 512), (2048, NS)]
NEXP = 8
DFE = 2816
FCH = DFE // 128


class Sem:
    _n = 0

    def __init__(self, h):
        self.h = h
        Sem._n += 1
        self.id = Sem._n
        self.total = 0


class Ev:
    __slots__ = ("sem", "val", "eng")

    def __init__(self, sem, val, eng):
        self.sem, self.val, self.eng = sem, val, eng


class Dep:
    __slots__ = ("w", "r", "dsem", "psum")

    def __init__(self):
        self.w = {}
        self.r = {}
        self.dsem = None
        self.psum = False


class View:
    __slots__ = ("ap", "dep", "sb")

    def __init__(self, ap, dep, sb):
        self.ap, self.dep, self.sb = ap, dep, sb

    def __getitem__(self, key):
        return View(self.ap[key], self.dep, self.sb)

    def rr(self, pat, **kw):
        return View(self.ap.rearrange(pat, **kw), self.dep, self.sb)

    def bc(self, shape):
        return View(self.ap.to_broadcast(shape), self.dep, self.sb)


ENG_ATTR = dict(pe="tensor", act="scalar", dve="vector", pool="gpsimd", sp="sync")
EPOCH = 20000


class KB:
    def __init__(self, nc):
        self.nc = nc
        self.root = ExitStack()
        self.stack = [self.root]
        self.eng = {k: getattr(nc, v) for k, v in ENG_ATTR.items()}
        self.esem = {}
        self.tick = {}
        self.waited = {k: {} for k in ENG_ATTR}
        self.all_esems = []
        self.dsems = []
        self.final = {}
        self.ncnt = 0
        for k in ENG_ATTR:
            self._new_esem(k)
        self.dfree = []
        self.scope_ds = [[]]
        self.gsem = self._new_dsem()

    def _name(self, base):
        self.ncnt += 1
        return f"{base}_{self.ncnt}"

    def _new_esem(self, k):
        s = Sem(self.root.enter_context(self.nc.semaphore(self._name("es" + k))))
        self.esem[k] = s
        self.tick[k] = 0
        self.all_esems.append((k, s))

    def _new_dsem(self):
        if getattr(self, "dfree", None):
            s = self.dfree.pop()
        else:
            s = Sem(self.root.enter_context(self.nc.semaphore(self._name("ds"))))
            self.dsems.append(s)
        if hasattr(self, "scope_ds"):
            self.scope_ds[-1].append(s)
        return s

    def sb(self, shape, dt, name="t"):
        t = self.stack[-1].enter_context(self.nc.sbuf_tensor(self._name(name), list(shape), dt))
        return View(t[tuple(slice(None) for _ in shape)], Dep(), True)

    def ps(self, shape, dt, name="ps"):
        t = self.stack[-1].enter_context(self.nc.psum_tensor(self._name(name), list(shape), dt))
        d = Dep()
        d.psum = True
        return View(t[tuple(slice(None) for _ in shape)], d, True)

    def dram(self, name, shape, dt, kind):
        t = self.nc.dram_tensor(name, list(shape), dt, kind=kind)
        return View(t.ap(), Dep() if kind == "Internal" else None, False)

    def scope(self):
        kb = self

        class _S:
            def __enter__(s):
                es = ExitStack()
                kb.stack.append(es)
                kb.scope_ds.append([])
                return es

            def __exit__(s, *a):
                if getattr(kb, "finished", False):
                    return False
                kb.barrier()
                kb.stack.pop().close()
                kb.dfree.extend(kb.scope_ds.pop())
                return False

        return _S()

    def _wait(self, eng, need):
        e = self.eng[eng]
        wd = self.waited[eng]
        for ev in need.values():
            if wd.get(ev.sem.id, 0) < ev.val:
                e.wait_ge(ev.sem.h, ev.val)
                wd[ev.sem.id] = ev.val

    @staticmethod
    def _add(need, ev):
        o = need.get(ev.sem.id)
        if o is None or o.val < ev.val:
            need[ev.sem.id] = ev

    def op(self, eng, fn, R=(), W=()):
        need = {}
        wdeps = [v.dep for v in W if v is not None and v.dep is not None]
        rdeps = [v.dep for v in R if v is not None and v.dep is not None]
        for d in rdeps:
            for ev in d.w.values():
                self._add(need, ev)
            if d.psum:
                for ev in d.r.values():
                    if ev.eng != eng:
                        self._add(need, ev)
        for d in wdeps:
            for ev in d.w.values():
                if ev.eng != eng:
                    self._add(need, ev)
            for ev in d.r.values():
                if ev.eng != eng:
                    self._add(need, ev)
        self._wait(eng, need)
        if self.tick[eng] >= EPOCH:
            self._new_esem(eng)
        ins = fn(self.eng[eng])
        sem = self.esem[eng]
        self.tick[eng] += 1
        ins.then_inc(sem.h, 1)
        ev = Ev(sem, self.tick[eng], eng)
        for d in wdeps:
            d.w = {sem.id: ev}
            d.r = {}
        for d in rdeps:
            if d not in wdeps:
                d.r[sem.id] = ev
        return ins

    def dma(self, q, out, in_, acc=False, fn=None, final=False, R=()):
        need = {}
        od, idp = out.dep, in_.dep
        xdeps = [v.dep for v in R if v.dep is not None]
        for d in xdeps:
            for ev in d.w.values():
                self._add(need, ev)
        if out.sb and od is not None:
            owner = od
        elif in_.sb and idp is not None:
            owner = idp
        else:
            owner = None
        if owner is not None:
            if owner.dsem is None:
                owner.dsem = self._new_dsem()
            dsem = owner.dsem
        else:
            dsem = self.gsem
        if idp is not None:
            for ev in idp.w.values():
                self._add(need, ev)
        if od is not None:
            for ev in od.w.values():
                if acc or (ev.eng == "dma" and ev.sem is dsem):
                    continue
                self._add(need, ev)
            for ev in od.r.values():
                self._add(need, ev)
        self._wait(q, need)
        if fn is None:
            ins = self.eng[q].dma_start(out=out.ap, in_=in_.ap)
        else:
            ins = fn(self.eng[q])
        ins.then_inc(dsem.h, 16)
        dsem.total += 16
        ev = Ev(dsem, dsem.total, "dma")
        if od is not None:
            if acc:
                od.w[dsem.id] = ev
            else:
                od.w = {dsem.id: ev}
            od.r = {}
        if idp is not None:
            idp.r[dsem.id] = ev
        for d in xdeps:
            d.r[dsem.id] = ev
        if final or (od is None and not out.sb):
            self.final[dsem.id] = ev
        return ins

    def barrier(self):
        for k in ENG_ATTR:
            need = {}
            for k2, s in self.all_esems:
                if k2 != k:
                    v = self.tick[k2] if s is self.esem[k2] else EPOCH
                    if v > 0:
                        self._add(need, Ev(s, v, k2))
            for s in self.dsems:
                if s.total > 0:
                    self._add(need, Ev(s, s.total, "dma"))
            self._wait(k, need)

    def finish(self):
        self._wait("sp", dict(self.final))
        self.barrier()
        self.finished = True
        while len(self.stack) > 1:
            self.stack.pop().close()
        self.root.close()

    def mm(self, out, lhsT, rhs, start=True, stop=True):
        return self.op("pe", lambda e: e.matmul(out.ap, lhsT.ap, rhs.ap, start=start, stop=stop),
                       R=[lhsT, rhs], W=[out])

    def tr(self, out, in_, ident):
        return self.op("pe", lambda e: e.transpose(out.ap, in_.ap, ident.ap), R=[in_, ident], W=[out])

    def act(self, out, in_, func, bias=None, scale=None, accum=None, eng="act"):
        kw = {}
        R = [in_]
        if bias is not None:
            if isinstance(bias, View):
                kw["bias"] = bias.ap
                R.append(bias)
            else:
                kw["bias"] = bias
        if scale is not None:
            if isinstance(scale, View):
                kw["scale"] = scale.ap
                R.append(scale)
            else:
                kw["scale"] = scale
        W = [out]
        if accum is not None:
            kw["accum_out"] = accum.ap
            W.append(accum)
        return self.op(eng, lambda e: e.activation(out=out.ap, in_=in_.ap, func=func, **kw), R=R, W=W)

    def ts(self, eng, out, in0, s1, s2=None, op0=ALU.mult, op1=None, accum=None):
        R = [in0]
        a1 = s1
        if isinstance(s1, View):
            a1 = s1.ap
            R.append(s1)
        a2 = s2
        if isinstance(s2, View):
            a2 = s2.ap
            R.append(s2)
        kw = {}
        if op1 is not None:
            kw["op1"] = op1
        W = [out]
        if accum is not None:
            kw["accum_out"] = accum.ap
            W.append(accum)
        return self.op(eng, lambda e: e.tensor_scalar(out.ap, in0.ap, a1, a2, op0, **kw), R=R, W=W)

    def tt(self, eng, out, in0, in1, op):
        return self.op(eng, lambda e: e.tensor_tensor(out.ap, in0.ap, in1.ap, op), R=[in0, in1], W=[out])

    def stt(self, eng, out, in0, scalar, in1, op0, op1):
        R = [in0, in1]
        sc = scalar
        if isinstance(scalar, View):
            sc = scalar.ap
            R.append(scalar)
        return self.op(eng, lambda e: e.scalar_tensor_tensor(out.ap, in0.ap, sc, in1.ap, op0, op1), R=R, W=[out])

    def cp(self, eng, out, in_):
        if eng == "act":
            return self.op("act", lambda e: e.copy(out.ap, in_.ap), R=[in_], W=[out])
        return self.op(eng, lambda e: e.tensor_copy(out.ap, in_.ap), R=[in_], W=[out])

    def red(self, eng, out, in_, op=ALU.add, axis=None):
        ax = axis if axis is not None else AX.X
        return self.op(eng, lambda e: e.tensor_reduce(out.ap, in_.ap, ax, op), R=[in_], W=[out])

    def memset(self, eng, out, val):
        return self.op(eng, lambda e: e.memset(out.ap, val), R=[], W=[out])

    def recip(self, out, in_):
        return self.op("dve", lambda e: e.reciprocal(out.ap, in_.ap), R=[in_], W=[out])


def _load_rows_T(kb, psrot, IDENT, dst, src_rows, n):
    tmp = kb.sb([128, 128], F32, "lrt")
    kb.dma("sp", tmp[0:n, :], src_rows)
    ps = psrot.next()
    kb.tr(ps[:, 0:n], tmp[0:n, :], IDENT[0:n, 0:n])
    kb.cp("dve", dst, ps[:, 0:n])


class Rot:
    def __init__(self, items):
        self.items = items
        self.i = 0

    def next(self):
        v = self.items[self.i % len(self.items)]
        self.i += 1
        return v


CF = {}
_o = 0
for _n, _w in [("ident", 128), ("tri", 128), ("ones", 128), ("sufx", 128), ("tri64", 128), ("sel64", 128),
               ("dmat", 512), ("xi", 4), ("zeta", 4), ("pfix", 64), ("bd", 512), ("sel8", 1024)]:
    CF[_n] = (_o, _w)
    _o += _w
NCF = _o
NTILE = 18


def make_consts():
    c = np.zeros((128, NCF), np.float32)
    a = np.arange(128)

    def put(name, m):
        o, w = CF[name]
        c[: m.shape[0], o:o + m.shape[1]] = m

    put("ident", np.eye(128, dtype=np.float32))
    put("tri", (a[:, None] <= a[None, :]).astype(np.float32))
    put("ones", np.ones((128, 128), np.float32))
    put("sufx", (a[:, None] > a[None, :]).astype(np.float32))
    same = (a[:, None] // 64) == (a[None, :] // 64)
    put("tri64", (same & (a[:, None] <= a[None, :])).astype(np.float32))
    put("sel64", (a[:, None] == (a[None, :] // 64) * 64 + 63).astype(np.float32))
    logg = np.log1p(-np.exp2(-5.0 - np.arange(4, dtype=np.float32))).astype(np.float32)
    n = np.arange(128, dtype=np.float32)
    diff = n[None, :] - n[:, None]
    dm = np.zeros((128, 512), np.float32)
    for h in range(4):
        dm[:, h * 128:(h + 1) * 128] = np.where(diff >= 0, np.exp(np.where(diff >= 0, diff, 0.0) * logg[h]), 0.0)
    put("dmat", dm)
    put("xi", np.exp((n[:, None] + 1.0) * logg[None, :]).astype(np.float32))
    put("zeta", np.exp((127.0 - n)[:, None] * logg[None, :]).astype(np.float32))
    pf = np.ones((128, 64), np.float32)
    for g, w in enumerate((2, 4, 8, 16)):
        t = np.arange(16)
        pf[:, g * 16:(g + 1) * 16] = (np.float32(w) / np.minimum(w, t + 1).astype(np.float32))[None, :]
    put("pfix", pf)
    bd = np.zeros((4, 512), np.float32)
    for h in range(4):
        bd[h, h * 128:(h + 1) * 128] = 1.0
    put("bd", bd)
    s8 = np.zeros((8, 1024), np.float32)
    for e in range(8):
        s8[e, e * 128:(e + 1) * 128] = 1.0
    put("sel8", s8)
    half = 64
    freqs = (np.float32(10000.0) ** (-np.arange(half, dtype=np.float32) / np.float32(half))).astype(np.float32)
    rot = np.zeros((128, 2, NTILE, 128), np.float32)
    for ti in range(NTILE):
        pos = (ti * 128 + a).astype(np.float32) if ti < 16 else np.full(128, PAST, np.float32)
        ang = (pos[:, None] * freqs[None, :]).astype(np.float32).astype(np.float64)
        cs, sn = np.cos(ang).astype(np.float32), np.sin(ang).astype(np.float32)
        rot[:, 0, ti, :64] = cs
        rot[:, 0, ti, 64:] = cs
        rot[:, 1, ti, :64] = -sn
        rot[:, 1, ti, 64:] = sn
    gam = np.exp(logg.astype(np.float64))
    gc = np.exp(128.0 * logg.astype(np.float64))
    return c, rot.reshape(128, 2 * NTILE * 128), [float(x) for x in gam], [float(x) for x in gc]


def build_program(depth=DEPTH, n_pool=N_POOL, stop_after=None):
    nc = bass.Bass("TRN2", target_bir_lowering=False)
    kb = KB(nc)
    _, _, GAM, GC = make_consts()

    def ein(name, shape, dt=F32):
        return kb.dram(name, shape, dt, "ExternalInput")

    def eout(name, shape):
        return kb.dram(name, shape, F32, "ExternalOutput")

    xp = ein("xp", [T, D])
    xs = ein("xs", [NS, D])
    crows = ein("crows", [3, D])
    ck = ein("ck", [n_pool * 2 * 128, 512])
    cv = ein("cv", [n_pool * 2 * 128, 512])
    clf = ein("clf", [n_pool * 2, 512])
    pt = ein("pt", [NS, NPAGE], I32)
    spool = ein("spool", [NS, DEPTH, 15, 512])
    sret = ein("sret", [NS, DEPTH, 4, 128, 128])
    sgla = ein("sgla", [NS, DEPTH, 4, 64, 128])
    constf = ein("constf", [128, NCF])
    rotc = ein("rotc", [128, 2 * NTILE * 128])
    Wt = []
    for l in range(depth):
        w = dict(
            ada=ein(f"w_ada{l}", [D, 6 * D]), bada=ein(f"b_ada{l}", [96, 128]),
            win=ein(f"w_in{l}", [D, D_IN]), bfox=ein(f"b_fox{l}", [1, 4]),
            wa2=ein(f"w_a2{l}", [16, 256]), ba=ein(f"b_a{l}", [1, 256]),
            wmap=ein(f"w_map{l}", [4, 128, 128]), pscale=ein(f"pscale{l}", [4, 128]),
            wbr=ein(f"w_br{l}", [4, 512, D]), wo=ein(f"w_o{l}", [D, D]),
            ln=ein(f"ln{l}", [64, 128]),
        )
        if l % 2 == 0:
            w["fg"] = ein(f"w_fg{l}", [D, 2 * DFE])
            w["fu"] = ein(f"w_fu{l}", [D, 2 * DFE])
            w["fd"] = ein(f"w_fd{l}", [2 * DFE, D])
        else:
            w["wr"] = ein(f"w_r{l}", [D, 8])
            w["br"] = ein(f"b_r{l}", [8, 1])
            w["eg"] = ein(f"w_eg{l}", [NEXP, D, DFE])
            w["eu"] = ein(f"w_eu{l}", [NEXP, D, DFE])
            w["ed"] = ein(f"w_ed{l}", [NEXP, DFE, D])
        Wt.append(w)

    y_p = eout("y_p", [T, D])
    y_s = eout("y_s", [NS, D])
    nk_p = eout("nk_p", [DEPTH, T, 512])
    nv_p = eout("nv_p", [DEPTH, T, 512])
    nlf_p = eout("nlf_p", [DEPTH, T, 4])
    npool_p = eout("npool_p", [DEPTH, 15, 512])
    nret_p = eout("nret_p", [DEPTH, 4, 128, 128])
    ngla_p = eout("ngla_p", [DEPTH, 4, 64, 128])
    nk_s = eout("nk_s", [NS, DEPTH, 512])
    nv_s = eout("nv_s", [NS, DEPTH, 512])
    nlf_s = eout("nlf_s", [NS, DEPTH, 4])
    npool_s = eout("npool_s", [NS, DEPTH, 15, 512])
    nret_s = eout("nret_s", [NS, DEPTH, 4, 128, 128])
    ngla_s = eout("ngla_s", [NS, DEPTH, 4, 64, 128])

    xaT = kb.dram("xaT", [KC, 128, TT], F32, "Internal")
    ytS = kb.dram("ytS", [16, 128, TT], BF16, "Internal")
    mgS = kb.dram("mgS", [NB, 128, KC, BW], BF16, "Internal")

    CFt = kb.sb([128, NCF], F32, "constf")
    kb.dma("sp", CFt, constf)

    def cf(name, rows=128, lo=0, hi=None):
        o, w = CF[name]
        hi = w if hi is None else hi
        return CFt[0:rows, o + lo:o + hi]

    IDENT = cf("ident")
    ONESF = cf("ones")
    TRI = cf("tri")
    ONESB = kb.sb([128, 128], BF16, "onesb")
    kb.memset("dve", ONESB, 1.0)
    PS = [kb.ps([128, 512], F32, f"bank{i}") for i in range(8)]
    psrot = Rot(PS)
    MOD = [kb.sb([128, 6, KC, 3], F32, f"mod{l}") for l in range(depth)]
    LNP = [kb.sb([128, 4, KC], F32, f"lnp{l}") for l in range(depth)]

    def load_rows_T(dst, src_rows, n):
        return _load_rows_T(kb, psrot, IDENT, dst, src_rows, n)

    def _unused(dst, src_rows, n):
        tmp = kb.sb([128, 128], F32, "lrt")
        kb.dma("sp", tmp[0:n, :], src_rows)
        ps = psrot.next()
        kb.tr(ps[:, 0:n], tmp[0:n, :], IDENT[0:n, 0:n])
        kb.cp("dve", dst, ps[:, 0:n])

    with kb.scope():
        CR = kb.sb([3, D], F32, "crows")
        kb.dma("sp", CR, crows)
        SCR = kb.sb([3, D], F32, "scr")
        kb.act(SCR, CR, AF.Silu)
        scT = kb.sb([128, KC, 3], BF16, "scT")
        ps = psrot.next()
        for k in range(KC):
            kb.tr(ps[:, k * 3:(k + 1) * 3], SCR[:, k * 128:(k + 1) * 128], IDENT[0:3, 0:3])
        kb.cp("dve", scT.rr("p k r -> p (k r)"), ps[:, 0:KC * 3])
        slabs = Rot([kb.sb([128, KC, 1024], BF16, f"adaslab{i}") for i in range(2)])
        for l in range(depth):
            BAD = kb.sb([128, 96], F32, "bada")
            load_rows_T(BAD, Wt[l]["bada"], 96)
            load_rows_T(LNP[l].rr("p a k -> p (a k)"), Wt[l]["ln"], 64)
            wv = Wt[l]["ada"].rr("(k p) n -> p k n", p=128)
            for g in range(12):
                sl = slabs.next()
                kb.dma("pool", sl, wv[:, :, g * 1024:(g + 1) * 1024])
                ps = psrot.next()
                for j in range(8):
                    for k in range(KC):
                        kb.mm(ps[:, j * 4:j * 4 + 3], sl[:, k, j * 128:(j + 1) * 128], scT[:, k, :],
                              start=(k == 0), stop=(k == KC - 1))
                part, kk0 = (g * 8) // 16, (g * 8) % 16
                for r in range(3):
                    kb.tt("dve", MOD[l][:, part, kk0:kk0 + 8, r],
                          ps[:, 0:32].rr("p (j q) -> p j q", q=4)[:, :, r], BAD[:, g * 8:(g + 1) * 8], ALU.add)
            for part in (1, 4):
                kb.ts("dve", MOD[l][:, part], MOD[l][:, part], 1.0, None, op0=ALU.add)
    if stop_after == "p0":
        dbg = eout("dbg", [128, 6 * KC * 3])
        kb.dma("sp", dbg, MOD[0].rr("p a k r -> p (a k r)"))
        kb.finish()
        return nc
    return _build_layers(kb, locals())


def weight_inputs(z, depth=DEPTH):
    w = {}
    for l in range(depth):
        w[f"w_ada{l}"] = z["w_ada"][l]
        w[f"b_ada{l}"] = z["b_ada"][l].reshape(96, 128)
        w[f"w_in{l}"] = z["w_in"][l]
        w[f"b_fox{l}"] = z["b_fox_f"][l].reshape(1, 4)
        w[f"w_a2{l}"] = z["w_gla_a2"][l]
        w[f"b_a{l}"] = z["b_gla_a"][l].reshape(1, 256)
        w[f"w_map{l}"] = z["w_pool_map"][l]
        w[f"pscale{l}"] = z["pool_scale"][l].reshape(4, 128)
        w[f"w_br{l}"] = z["w_branch"][l]
        w[f"w_o{l}"] = z["w_o"][l]
        w[f"ln{l}"] = np.concatenate([z["ln1_g"][l], z["ln1_b"][l], z["ln2_g"][l], z["ln2_b"][l]]).reshape(64, 128)
        j = l // 2
        if l % 2 == 0:
            w[f"w_fg{l}"] = z["w_ff_gate"][j]
            w[f"w_fu{l}"] = z["w_ff_up"][j]
            w[f"w_fd{l}"] = z["w_ff_down"][j]
        else:
            w[f"w_r{l}"] = z["w_router"][j]
            w[f"b_r{l}"] = z["b_router"][j].reshape(8, 1)
            w[f"w_eg{l}"] = z["w_ex_gate"][j]
            w[f"w_eu{l}"] = z["w_ex_up"][j]
            w[f"w_ed{l}"] = z["w_ex_down"][j]
    return w


def _build_layers(kb, g):
    nc = g["nc"]
    depth, n_pool, stop_after = g["depth"], g["n_pool"], g["stop_after"]
    Wt, MOD, LNP, PS, psrot, cf = g["Wt"], g["MOD"], g["LNP"], g["PS"], g["psrot"], g["cf"]
    IDENT, ONESF, TRI, ONESB = g["IDENT"], g["ONESF"], g["TRI"], g["ONESB"]
    GAM, GC = g["GAM"], g["GC"]
    xp, xs, xaT, ytS, mgS = g["xp"], g["xs"], g["xaT"], g["ytS"], g["mgS"]
    eout = g["eout"]
    evq = Rot(["dve", "act"])

    def evac(out, in_, scale=None):
        e = evq.next()
        if scale is None:
            kb.cp(e, out, in_)
        elif e == "dve":
            kb.ts("dve", out, in_, float(scale), None, op0=ALU.mult)
        else:
            kb.act(out, in_, AF.Copy, scale=float(scale))

    def tok_range(ti):
        if ti < 16:
            return ti * 128, 128
        return T + (ti - 16), 1

    for l in range(depth):
        W = Wt[l]
        last = (l == depth - 1)
        winv = W["win"].rr("(k p) n -> p k n", p=128)
        with kb.scope():
            UT = kb.sb([128, KC, TT], BF16, "UT")
            with kb.scope():
                if l == 0:
                    xts = Rot([kb.sb([128, D], F32, f"xt{i}") for i in range(2)])
                    xas = Rot([kb.sb([128, KC, 128], F32, f"xa{i}") for i in range(2)])
                    import os
                    for ti in [int(v) for v in os.environ.get('KDEV_TILES', ','.join(str(i) for i in range(17))).split(',') if v != '']:
                        xt = xts.next()
                        xa = xas.next()
                        m = 128 if ti < 16 else NS
                        c0 = ti * 128
                        kb.dma("sp", xt[0:m, :], xp[ti * 128:(ti + 1) * 128, :] if ti < 16 else xs)
                        for q4 in range(4):
                            ps = psrot.next()
                            for qq in range(4):
                                e = q4 * 4 + qq
                                kb.tr(ps[:, qq * 128:qq * 128 + m], xt[0:m, e * 128:(e + 1) * 128], IDENT[0:m, 0:m])
                            for qq in range(4):
                                e = q4 * 4 + qq
                                if os.environ.get("KDEV_NOACT"):
                                    continue
                                if os.environ.get("KDEV_ACTF"):
                                    kb.act(UT[:, e, c0:c0 + 128], ps[:, qq * 128:(qq + 1) * 128], AF.Identity, bias=0.5, scale=2.0)
                                elif ti < 16:
                                    kb.act(UT[:, e, c0:c0 + 128], ps[:, qq * 128:(qq + 1) * 128], AF.Identity,
                                           bias=MOD[l][:, 0, e, 0:1], scale=MOD[l][:, 1, e, 0:1])
                                else:
                                    for r in range(NS):
                                        kb.act(UT[:, e, T + r:T + r + 1], ps[:, qq * 128 + r:qq * 128 + r + 1], AF.Identity,
                                               bias=MOD[l][:, 0, e, 1 + r:2 + r], scale=MOD[l][:, 1, e, 1 + r:2 + r])
                            kb.ts("dve", xa[:, q4 * 4:(q4 + 1) * 4, 0:m],
                                  ps.rr("p (q c) -> p q c", c=128)[:, :, 0:m], ALPHA, None, op0=ALU.mult)
                        if not os.environ.get("KDEV_NOXA"):
                            kb.dma("sp", xaT[:, :, c0:c0 + m].rr("k p c -> p k c"), xa[:, :, 0:m], acc=True)
                else:
                    SCA = kb.sb([128, KC, 3], F32, "sca")
                    kb.ts("dve", SCA, MOD[l][:, 1], 1.0 / ALPHA, None, op0=ALU.mult)
                    xcs = Rot([kb.sb([128, TT], F32, f"xc{i}") for i in range(2)])
                    for e in range(KC):
                        xc = xcs.next()
                        kb.dma("sp", xc, xaT[e])
                        kb.act(UT[:, e, 0:T], xc[:, 0:T], AF.Identity, bias=MOD[l][:, 0, e, 0:1], scale=SCA[:, e, 0:1])
                        for r in range(NS):
                            kb.act(UT[:, e, T + r:T + r + 1], xc[:, T + r:T + r + 1], AF.Identity,
                                   bias=MOD[l][:, 0, e, 1 + r:2 + r], scale=SCA[:, e, 1 + r:2 + r])
            if stop_after == "pA":
                dbg = eout("dbg", [KC, 128, TT])
                DB = kb.sb([128, TT], F32, "dbgt")
                for e in range(KC):
                    kb.cp("dve", DB, UT[:, e, :])
                    kb.dma("sp", dbg[e], DB)
                kb.finish()
                return nc

            slab_pool = Rot([kb.sb([128, KC, 512], BF16, f"slab{i}") for i in range(2)])

            def load_slab(col0, ncols):
                sl = slab_pool.next()
                kb.dma("pool", sl[:, :, 0:ncols], winv[:, :, col0:col0 + ncols])
                return sl

            def proj_tm(col0, ncols, consume, tiles=range(NTILE)):
                sl = load_slab(col0, ncols)
                for ti in tiles:
                    c0, m = tok_range(ti)
                    ps = psrot.next()
                    for k in range(KC):
                        kb.mm(ps[0:m, 0:ncols], UT[:, k, c0:c0 + m], sl[:, k, 0:ncols], start=(k == 0), stop=(k == KC - 1))
                    consume(ti, m, ps)

            def proj_fm(col0, ncols, consume):
                sl = load_slab(col0, ncols)
                for (c0, cn) in COLT:
                    ps = psrot.next()
                    for k in range(KC):
                        kb.mm(ps[0:ncols, 0:cn], sl[:, k, 0:ncols], UT[:, k, c0:c0 + cn], start=(k == 0), stop=(k == KC - 1))
                    consume(c0, cn, ps)

            def store_y(chunk, Y):
                kb.dma("sp", ytS[chunk], Y, acc=True)

            _mixer_fox(kb, locals(), g)
            if stop_after == "fox":
                dbgy = kb.dram("dbg_y", [16, 128, TT], BF16, "ExternalOutput")
                kb.dma("sp", dbgy, ytS)
                kb.finish()
                return nc
            _mixer_pool(kb, locals(), g)
            if stop_after == "pool":
                dbgy = kb.dram("dbg_y", [16, 128, TT], BF16, "ExternalOutput")
                kb.dma("sp", dbgy, ytS)
                kb.finish()
                return nc
            _mixer_ret(kb, locals(), g)
            if stop_after == "ret":
                dbgy = kb.dram("dbg_y", [16, 128, TT], BF16, "ExternalOutput")
                kb.dma("sp", dbgy, ytS)
                kb.finish()
                return nc
            _mixer_gla(kb, locals(), g)
            if stop_after == "gla":
                dbgy = kb.dram("dbg_y", [16, 128, TT], BF16, "ExternalOutput")
                kb.dma("sp", dbgy, ytS)
                kb.finish()
                return nc
            _phase_merge(kb, locals(), g)
            if stop_after == "merge":
                dbgm = kb.dram("dbg_m", [NB, 128, KC, BW], BF16, "ExternalOutput")
                kb.dma("sp", dbgm, mgS)
                kb.finish()
                return nc
        _phase_ffn(kb, locals(), g)
    kb.finish()
    return nc


def _mixer_fox(kb, L, g):
    l, W, UT = L["l"], L["W"], L["UT"]
    proj_tm, proj_fm, evac, store_y, psrot = L["proj_tm"], L["proj_fm"], L["evac"], L["store_y"], g["psrot"]
    cf, IDENT, ONESF, TRI, ONESB, PS = g["cf"], g["IDENT"], g["ONESF"], g["TRI"], g["ONESB"], g["PS"]
    nk_p, nv_p, nlf_p, nk_s, nv_s, nlf_s = g["nk_p"], g["nv_p"], g["nlf_p"], g["nk_s"], g["nv_s"], g["nlf_s"]
    ck, cv, clf, pt = g["ck"], g["cv"], g["clf"], g["pt"]
    SCALE = 128.0 ** -0.5
    with kb.scope():
        VT = kb.sb([128, 16, 512], BF16, "VT")
        KNEW = [kb.sb([1, 512], F32, f"knew{r}") for r in range(NS)]
        VNEW = [kb.sb([1, 512], F32, f"vnew{r}") for r in range(NS)]
        QROW = [kb.sb([1, 512], F32, f"qrow{r}") for r in range(NS)]
        LFR = kb.sb([128, NTILE, 4], F32, "lfraw")
        kb.memset("dve", LFR, 0.0)
        stg = Rot([kb.sb([128, 512], F32, f"stg{i}") for i in range(2)])

        def k_cons(ti, m, ps):
            if ti < 16:
                s = stg.next()
                evac(s[0:m, :], ps[0:m, 0:512])
                kb.dma("sp", nk_p[l, ti * 128:(ti + 1) * 128, :], s[0:m, :])
            else:
                r = ti - 16
                kb.cp("dve", KNEW[r], ps[0:1, 0:512])
                kb.dma("sp", nk_s[r, l:l + 1, :], KNEW[r])

        def v_cons(ti, m, ps):
            if ti < 16:
                s = stg.next()
                evac(s[0:m, :], ps[0:m, 0:512])
                kb.dma("sp", nv_p[l, ti * 128:(ti + 1) * 128, :], s[0:m, :])
                kb.cp("act", VT[:, ti, :], ps[:, 0:512])
            else:
                r = ti - 16
                kb.cp("dve", VNEW[r], ps[0:1, 0:512])
                kb.dma("sp", nv_s[r, l:l + 1, :], VNEW[r])

        proj_tm(OFF["fk"], 512, k_cons)
        proj_tm(OFF["fv"], 512, v_cons)
        proj_tm(OFF["ff"], 4, lambda ti, m, ps: kb.cp("dve", LFR[0:m, ti, :], ps[0:m, 0:4]))
        proj_tm(OFF["fq"], 512, lambda ti, m, ps: kb.ts("dve", QROW[ti - 16], ps[0:1, 0:512], SCALE, None, op0=ALU.mult),
                tiles=range(16, NTILE))

        BF4 = kb.sb([128, 4], F32, "bf4")
        kb.dma("sp", BF4, W["bfox"].bc([128, 4]))
        LF = kb.sb([128, NTILE, 4], F32, "lf")
        kb.tt("dve", LF, LFR, BF4.rr("p (o h) -> p o h", o=1).bc([128, NTILE, 4]), ALU.add)
        kb.act(LF, LF, AF.Exp, scale=-1.0)
        kb.act(LF, LF, AF.Ln, bias=1.0)
        kb.ts("dve", LF, LF, -1.0, None, op0=ALU.mult)
        kb.dma("sp", nlf_p[l].rr("(i p) h -> p i h", p=128), LF[:, 0:16, :])
        for r in range(NS):
            kb.dma("sp", nlf_s[r, l:l + 1, :], LF[0:1, 16 + r, :])
        LF2 = LF[:, 0:16, :].rr("p i h -> p (i h)")
        psA = psrot.next()
        kb.mm(psA[:, 0:64], TRI, LF2)
        psB = psrot.next()
        kb.mm(psB[:, 0:64], ONESF, LF2)
        TOT = kb.sb([128, 16, 4], F32, "tot")
        kb.cp("dve", TOT.rr("p i h -> p (i h)"), psB[:, 0:64])
        CI = kb.sb([128, 16, 4], F32, "ci")
        kb.cp("dve", CI[:, 0, :], TOT[:, 0, :])
        for i in range(1, 16):
            kb.tt("dve", CI[:, i, :], CI[:, i - 1, :], TOT[:, i, :], ALU.add)
        G = kb.sb([128, 16, 4], F32, "G")
        kb.tt("dve", G.rr("p i h -> p (i h)"), psA[:, 0:64], CI.rr("p i h -> p (i h)"), ALU.add)
        kb.tt("dve", G, G, TOT, ALU.subtract)

        QT = kb.sb([128, TT], BF16, "QT")
        KT = kb.sb([128, TT], BF16, "KT")
        BIAS = kb.sb([128, 16, 16], F32, "bias")
        YF = [kb.sb([128, TT], BF16, f"yf{h}") for h in range(4)]
        pts = Rot([kb.sb([128, 128], BF16, f"pt{i}") for i in range(4)])
        RL = kb.sb([128, 128], F32, "rl")
        sbanks = Rot([PS[0], PS[1]])
        obanks = Rot([PS[2], PS[3]])
        lbanks = Rot([PS[4], PS[5]])
        import os
        FOXLVL = int(os.environ.get("KDEV_FOX", "3"))
        for h in range(4 if FOXLVL >= 2 else 0):
            proj_fm(OFF["fq"] + h * 128, 128, lambda c0, cn, ps: evac(QT[:, c0:c0 + cn], ps[:, 0:cn], scale=SCALE))
            proj_fm(OFF["fk"] + h * 128, 128, lambda c0, cn, ps: evac(KT[:, c0:c0 + cn], ps[:, 0:cn]))
            for i in range(16):
                kb.ts("dve", BIAS[:, i, :], G[:, :, h], -1.0, CI[:, i, h:h + 1], op0=ALU.mult, op1=ALU.add)
            for i in range(16):
                ob = obanks.next()
                lb = lbanks.next()
                for j0 in range(0, i + 1, 4):
                    js = list(range(j0, min(j0 + 4, i + 1)))
                    sb = sbanks.next()
                    for jj, j in enumerate(js):
                        kb.mm(sb[:, jj * 128:(jj + 1) * 128], KT[:, j * 128:(j + 1) * 128], QT[:, i * 128:(i + 1) * 128])
                    for jj, j in enumerate(js):
                        p_ = pts.next()
                        kb.act(p_, sb[:, jj * 128:(jj + 1) * 128], AF.Exp, bias=BIAS[:, i, j:j + 1], scale=1.0)
                        if j == i:
                            kb.tt("pool", p_, p_, TRI, ALU.mult)
                        kb.mm(ob[:, 0:128], VT[:, j, h * 128:(h + 1) * 128], p_, start=(j == 0), stop=(j == i))
                        kb.mm(lb[:, 0:128], ONESB, p_, start=(j == 0), stop=(j == i))
                kb.recip(RL, lb[:, 0:128])
                kb.tt("dve", YF[h][:, i * 128:(i + 1) * 128], ob[:, 0:128], RL, ALU.mult)

        if FOXLVL >= 3:
            _fox_sample(kb, L, g, dict(KNEW=KNEW, VNEW=VNEW, QROW=QROW, LF=LF, YF=YF))
        for h in range(4):
            store_y(h, YF[h])


def _fox_sample(kb, L, g, S):
    l, W = L["l"], L["W"]
    psrot, cf, IDENT, ONESF, PS = g["psrot"], g["cf"], g["IDENT"], g["ONESF"], g["PS"]
    ck, cv, clf, pt, n_pool = g["ck"], g["cv"], g["clf"], g["pt"], g["n_pool"]
    KNEW, VNEW, QROW, LF, YF = S["KNEW"], S["VNEW"], S["QROW"], S["LF"], S["YF"]
    SUFX = cf("sufx")
    SCALE = 128.0 ** -0.5
    with kb.scope():
        IOTA = kb.sb([128, 1], F32, "iota")
        kb.op("pool", lambda e: e.iota(IOTA.ap, [[0, 1]], base=0, channel_multiplier=1,
                                       allow_small_or_imprecise_dtypes=True), W=[IOTA])
        kps = Rot([kb.sb([128, 512], F32, f"kp{i}") for i in range(2)])
        vps = Rot([kb.sb([128, 512], F32, f"vp{i}") for i in range(2)])
        prods = Rot([kb.sb([128, 512], F32, f"prod{i}") for i in range(2)])
        import os
        FSL = int(os.environ.get("KDEV_FS", "9"))
        for r in range(NS):
          with kb.scope():
                PTB = kb.sb([128, NPAGE], I32, "ptb")
                kb.dma("sp", PTB, pt[r:r + 1, :].bc([128, NPAGE]))
                PTF = kb.sb([128, NPAGE], F32, "ptf")
                kb.cp("dve", PTF, PTB)
                kb.ts("dve", PTF, PTF, 256.0, float(l * 128), op0=ALU.mult, op1=ALU.add)
                kb.ts("dve", PTF, PTF, IOTA[:, 0:1], None, op0=ALU.add)
                IDX = kb.sb([128, NPAGE], I32, "idx")
                kb.cp("dve", IDX, PTF)
                PTC = kb.sb([128, 1], I32, "ptc")
                kb.dma("sp", PTC, pt[r, :].rr("(p o) -> p o", o=1))
                PCF = kb.sb([128, 1], F32, "pcf")
                kb.cp("dve", PCF, PTC)
                kb.ts("dve", PCF, PCF, 2.0, float(l), op0=ALU.mult, op1=ALU.add)
                IDXQ = kb.sb([128, 1], I32, "idxq")
                kb.cp("dve", IDXQ, PCF)
                LFQ = kb.sb([128, 512], F32, "lfq")
                kb.dma("pool", LFQ, clf, fn=lambda e: e.indirect_dma_start(
                    out=LFQ.ap, out_offset=None, in_=clf.ap,
                    in_offset=bass.IndirectOffsetOnAxis(ap=IDXQ.ap, axis=0)), R=[IDXQ])
                if FSL < 2:
                    continue
                TOTQ = kb.sb([128, 4], F32, "totq")
                kb.red("dve", TOTQ, LFQ.rr("n (p h) -> n h p", h=4))
                psT = psrot.next()
                for h in range(4):
                    kb.tr(psT[:, h * 128:(h + 1) * 128], LFQ.rr("n (p h) -> n h p", h=4)[:, h, :], IDENT)
                LFP = kb.sb([128, 512], F32, "lfp")
                kb.cp("dve", LFP, psT)
                TOTB = kb.sb([128, 4, 128], F32, "totb")
                kb.cp("dve", TOTB, TOTQ.rr("n (h o) -> n h o", o=1).bc([128, 4, 128]))
                psD = psrot.next()
                for h in range(4):
                    kb.mm(psD[:, h * 128:(h + 1) * 128], SUFX, LFP[:, h * 128:(h + 1) * 128], start=True, stop=False)
                    kb.mm(psD[:, h * 128:(h + 1) * 128], TOTB[:, h, :], SUFX, start=False, stop=True)
                psQ = psrot.next()
                kb.mm(psQ, ONESF[0:1, :], QROW[r])
                QB = kb.sb([128, 512], F32, "qb")
                kb.cp("act", QB, psQ)
                psL = psrot.next()
                kb.mm(psL[:, 0:4], ONESF[0:1, :], LF[0:1, 16 + r, :])
                LFNB = kb.sb([128, 4], F32, "lfnb")
                kb.cp("dve", LFNB, psL[:, 0:4])
                if FSL < 4:
                    continue
                SC = kb.sb([128, 4, NPAGE], F32, "sc")
                for n in range(NPAGE):
                    kp = kps.next()
                    kb.dma("pool", kp, ck, fn=lambda e, kp=kp, n=n: e.indirect_dma_start(
                        out=kp.ap, out_offset=None, in_=ck.ap,
                        in_offset=bass.IndirectOffsetOnAxis(ap=IDX[:, n:n + 1].ap, axis=0)), R=[IDX])
                    pr = prods.next()
                    kb.tt("dve", pr, kp, QB, ALU.mult)
                    kb.red("dve", SC[:, :, n], pr.rr("p (h d) -> p h d", h=4))
                if FSL < 5:
                    continue
                SC2 = kb.sb([128, 4, NPAGE], F32, "sc2")
                kb.tt("dve", SC2.rr("p h n -> p (h n)"), SC.rr("p h n -> p (h n)"), psD, ALU.add)
                P = kb.sb([128, 4, NPAGE], F32, "P")
                for h in range(4):
                    kb.act(P[:, h, :], SC2[:, h, :], AF.Exp, bias=LFNB[:, h:h + 1], scale=1.0)
                SP = kb.sb([1, 512], F32, "sprod")
                kb.tt("dve", SP, QROW[r], KNEW[r], ALU.mult)
                WS = kb.sb([1, 4], F32, "ws")
                kb.red("dve", WS, SP.rr("p (h d) -> p h d", h=4))
                kb.act(WS, WS, AF.Exp)
                psS = psrot.next()
                kb.mm(psS[0:1, :], ONESF[:, 0:1], P.rr("p h n -> p (h n)"))
                LS = kb.sb([1, 4], F32, "ls")
                kb.red("dve", LS, psS[0:1, :].rr("p (h n) -> p h n", h=4))
                kb.tt("dve", LS, LS, WS, ALU.add)
                RLS = kb.sb([1, 4], F32, "rls")
                kb.recip(RLS, LS)
                if FSL < 6:
                    continue
                psO = psrot.next()
                for n in range(NPAGE):
                    vp = vps.next()
                    kb.dma("pool", vp, cv, fn=lambda e, vp=vp, n=n: e.indirect_dma_start(
                        out=vp.ap, out_offset=None, in_=cv.ap,
                        in_offset=bass.IndirectOffsetOnAxis(ap=IDX[:, n:n + 1].ap, axis=0)), R=[IDX])
                    kb.mm(psO[0:4, :], P[:, :, n], vp, start=(n == 0), stop=(n == NPAGE - 1))
                if FSL < 7:
                    continue
                OM = kb.sb([4, 512], F32, "om")
                kb.tt("dve", OM, psO[0:4, :], cf("bd", rows=4), ALU.mult)
                psR = psrot.next()
                kb.mm(psR[0:1, :], ONESF[0:4, 0:1], OM)
                YS = kb.sb([1, 512], F32, "ys")
                for h in range(4):
                    hs = slice(h * 128, (h + 1) * 128)
                    kb.stt("dve", YS[:, hs], VNEW[r][:, hs], WS[0:1, h:h + 1], psR[0:1, hs], ALU.mult, ALU.add)
                    kb.ts("dve", YS[:, hs], YS[:, hs], RLS[0:1, h:h + 1], None, op0=ALU.mult)
                psC = psrot.next()
                for h in range(4):
                    kb.mm(psC[:, h:h + 1], YS[0:1, h * 128:(h + 1) * 128], ONESF[0:1, 0:1])
                for h in range(4):
                    kb.cp("dve", YF[h][:, T + r:T + r + 1], psC[:, h:h + 1])


def _mixer_pool(kb, L, g):
    l, W, UT = L["l"], L["W"], L["UT"]
    proj_fm, evac, store_y, psrot = L["proj_fm"], L["evac"], L["store_y"], g["psrot"]
    cf, IDENT = g["cf"], g["IDENT"]
    npool_p, npool_s, spool = g["npool_p"], g["npool_s"], g["spool"]
    NE = 15 + T
    with kb.scope():
        WM = kb.sb([128, 4, 128], BF16, "wmap")
        kb.dma("pool", WM, W["wmap"].rr("g c d -> c g d"))
        PSC = kb.sb([128, 4], F32, "pscale")
        g["load_rows_T"](PSC, W["pscale"], 4)
        NP_P = kb.sb([15, 512], F32, "npoolp")
        NP_S = [kb.sb([1, 512], F32, f"npools{r}") for r in range(NS)]
        HS = [kb.sb([15, 512], F32, f"hist{r}") for r in range(NS)]
        for r in range(NS):
            kb.dma("sp", HS[r], spool[r, l])
            kb.dma("sp", npool_s[r, l, 0:14, :], spool[r, l, 1:15, :])
        RAW = kb.sb([128, NE], F32, "extR")
        EAB = [kb.sb([128, NE], F32, "extA"), kb.sb([128, NE], F32, "extB")]
        XR = [kb.sb([128, 16], F32, f"exts{r}") for r in range(NS)]
        XAB = [[kb.sb([128, 16], F32, f"extsa{r}") for r in range(NS)], [kb.sb([128, 16], F32, f"extsb{r}") for r in range(NS)]]
        DIFF = kb.sb([128, TT], BF16, "diff")
        YP = kb.sb([128, TT], BF16, "yp")
        TMP1 = kb.sb([128, 1], F32, "ptmp")
        kb.memset("pool", RAW[:, 0:15], 0.0)
        for gi, w in enumerate((2, 4, 8, 16)):
            def cons(c0, cn, ps):
                if c0 < T:
                    evac(RAW[:, 15 + c0:15 + c0 + cn], ps[:, 0:cn])
                else:
                    for r in range(NS):
                        kb.cp("dve", XR[r][:, 15:16], ps[:, r:r + 1])
            proj_fm(OFF["pin"] + gi * 128, 128, cons)
            for r in range(NS):
                ps = psrot.next()
                kb.tr(ps[:, 0:15], HS[r][:, gi * 128:(gi + 1) * 128], IDENT[0:15, 0:15])
                kb.cp("dve", XR[r][:, 0:15], ps[:, 0:15])
            ps = psrot.next()
            kb.tr(ps[0:15, 0:128], RAW[:, NE - 15:NE], IDENT)
            kb.cp("dve", NP_P[:, gi * 128:(gi + 1) * 128], ps[0:15, 0:128])
            for r in range(NS):
                ps = psrot.next()
                kb.tr(ps[0:1, 0:128], XR[r][:, 15:16], IDENT)
                kb.cp("dve", NP_S[r][:, gi * 128:(gi + 1) * 128], ps[0:1, 0:128])
            src, ssrc = RAW, XR
            s, i = 1, 0
            while s < w:
                dst, sdst = EAB[i % 2], XAB[i % 2]
                i += 1
                kb.tt("dve", dst[:, s:NE], src[:, s:NE], src[:, 0:NE - s], ALU.add)
                for r in range(NS):
                    kb.tt("pool", sdst[r][:, s:16], ssrc[r][:, s:16], ssrc[r][:, 0:16 - s], ALU.add)
                src, ssrc = dst, sdst
                s *= 2
            sc = EAB[i % 2]
            kb.ts("pool", sc[:, 15:NE], src[:, 15:NE], 1.0 / w, None, op0=ALU.mult)
            kb.tt("pool", sc[:, 15:31], sc[:, 15:31], cf("pfix", lo=gi * 16, hi=(gi + 1) * 16), ALU.mult)
            kb.tt("dve", DIFF[:, 0:T], sc[:, 15:NE], RAW[:, 15:NE], ALU.subtract)
            for r in range(NS):
                kb.ts("dve", TMP1, ssrc[r][:, 15:16], 1.0 / w, None, op0=ALU.mult)
                kb.tt("dve", DIFF[:, T + r:T + r + 1], TMP1, XR[r][:, 15:16], ALU.subtract)
            for (c0, cn) in COLT:
                ps = psrot.next()
                kb.mm(ps[:, 0:cn], WM[:, gi, :], DIFF[:, c0:c0 + cn])
                kb.ts("dve", YP[:, c0:c0 + cn], ps[:, 0:cn], PSC[:, gi:gi + 1], None, op0=ALU.mult)
            store_y(4 + gi, YP)
        kb.dma("sp", npool_p[l], NP_P)
        for r in range(NS):
            kb.dma("sp", npool_s[r, l, 14:15, :], NP_S[r])


def _norm_gate_T(kb, psrot, IDENT, O, m, gate, Ydst, rms, TMP):
    st = TMP["st"]
    if not rms:
        kb.red("dve", st[0:m, 0:1], O[0:m, :])
        kb.ts("dve", st[0:m, 0:1], st[0:m, 0:1], -1.0 / 128.0, None, op0=ALU.mult)
        kb.ts("dve", O[0:m, :], O[0:m, :], st[0:m, 0:1], None, op0=ALU.add)
    kb.memset("dve", st[0:m, 1:2], 0.0)
    kb.act(TMP["sq"][0:m, :], O[0:m, :], AF.Square, accum=st[0:m, 1:2])
    kb.act(st[0:m, 2:3], st[0:m, 1:2], AF.Ln, bias=LN_EPS, scale=1.0 / 128.0)
    kb.act(st[0:m, 2:3], st[0:m, 2:3], AF.Exp, scale=-0.5)
    kb.stt("dve", TMP["y"][0:m, :], O[0:m, :], st[0:m, 2:3], gate, ALU.mult, ALU.mult)
    ps = psrot.next()
    kb.tr(ps[:, 0:m], TMP["y"][0:m, :], IDENT[0:m, 0:m])
    kb.cp("act", Ydst, ps[:, 0:m])


def _mixer_ret(kb, L, g):
    l, W, UT = L["l"], L["W"], L["UT"]
    proj_tm, evac, store_y, psrot = L["proj_tm"], L["evac"], L["store_y"], g["psrot"]
    cf, IDENT, ONESF, PS = g["cf"], g["IDENT"], g["ONESF"], g["PS"]
    nret_p, nret_s, sret, rotc = g["nret_p"], g["nret_s"], g["sret"], g["rotc"]
    GAM, GC = g["GAM"], g["GC"]
    SCALE = 128.0 ** -0.5
    with kb.scope():
        ROT = kb.sb([128, 2, NTILE, 128], F32, "rot")
        kb.dma("sp", ROT.rr("p a t c -> p (a t c)"), rotc)
        RQ = kb.sb([128, NTILE, 128], F32, "rq")
        RK = kb.sb([128, NTILE, 128], F32, "rk")
        RV = kb.sb([128, 16, 128], BF16, "rv")
        SG = kb.sb([128, 16, 128], F32, "sg")
        RVS = [kb.sb([1, 128], F32, f"rvs{r}") for r in range(NS)]
        SGS = [kb.sb([1, 128], F32, f"sgs{r}") for r in range(NS)]
        QRT = kb.sb([128, T], BF16, "qrt")
        KRT = kb.sb([128, T], BF16, "krt")
        KZ = kb.sb([128, 16, 128], BF16, "kz")
        YR = kb.sb([128, TT], BF16, "yr")
        SF = kb.sb([128, 128], F32, "sf")
        SB = kb.sb([128, 128], BF16, "sbf")
        ATT = Rot([kb.sb([128, 128], BF16, f"att{i}") for i in range(2)])
        OT = Rot([kb.sb([128, 128], F32, f"ot{i}") for i in range(2)])
        BX = kb.sb([128, 128], F32, "bx")
        TMP = dict(st=kb.sb([128, 4], F32, "st"), sq=kb.sb([128, 128], F32, "sq"), y=kb.sb([128, 128], F32, "yn"))
        TA = kb.sb([128, 128], F32, "ta")
        TB = kb.sb([128, 128], F32, "tb")
        S0 = kb.sb([128, 128], F32, "s0")
        COL = kb.sb([128, 2], F32, "col")
        SC1 = kb.sb([1, 4], F32, "sc1")
        OS = kb.sb([1, 128], F32, "os")

        def rot_cons(dst):
            def f(ti, m, ps):
                kb.tt("dve", TA[0:m, :], ps[0:m, 0:128], ROT[0:m, 0, ti, :], ALU.mult)
                kb.tt("dve", TB[0:m, 0:64], ps[0:m, 64:128], ROT[0:m, 1, ti, 0:64], ALU.mult)
                kb.tt("dve", TB[0:m, 64:128], ps[0:m, 0:64], ROT[0:m, 1, ti, 64:128], ALU.mult)
                kb.tt("pool", dst[0:m, ti, :], TA[0:m, :], TB[0:m, :], ALU.add)
            return f

        def v_cons(ti, m, ps):
            if ti < 16:
                evac(RV[:, ti, :], ps[:, 0:128])
            else:
                kb.cp("dve", RVS[ti - 16], ps[0:1, 0:128])

        def g_cons(ti, m, ps):
            if ti < 16:
                kb.act(SG[:, ti, :], ps[:, 0:128], AF.Silu)
            else:
                kb.act(SGS[ti - 16], ps[0:1, 0:128], AF.Silu)

        for h in range(4):
            proj_tm(OFF["rq"] + h * 128, 128, rot_cons(RQ))
            proj_tm(OFF["rk"] + h * 128, 128, rot_cons(RK))
            proj_tm(OFF["rv"] + h * 128, 128, v_cons)
            proj_tm(OFF["rg"] + h * 128, 128, g_cons)
            for ti in range(16):
                ps = psrot.next()
                kb.tr(ps[:, 0:128], RQ[:, ti, :], IDENT)
                kb.tr(ps[:, 128:256], RK[:, ti, :], IDENT)
                kb.act(QRT[:, ti * 128:(ti + 1) * 128], ps[:, 0:128], AF.Copy, scale=SCALE)
                kb.cp("dve", KRT[:, ti * 128:(ti + 1) * 128], ps[:, 128:256])
                kb.ts("pool", KZ[:, ti, :], RK[:, ti, :], cf("zeta", lo=h, hi=h + 1), None, op0=ALU.mult)
            kb.memset("dve", SF, 0.0)
            kb.memset("dve", SB, 0.0)
            DM = cf("dmat", lo=h * 128, hi=(h + 1) * 128)
            XI = cf("xi", lo=h, hi=h + 1)
            for n in range(16):
                cs = slice(n * 128, (n + 1) * 128)
                psa = psrot.next()
                kb.mm(psa[:, 0:128], KRT[:, cs], QRT[:, cs])
                at = ATT.next()
                kb.tt("dve", at, psa[:, 0:128], DM, ALU.mult)
                pso = psrot.next()
                kb.mm(pso[:, 0:128], at, RV[:, n, :])
                kb.mm(pso[:, 128:256], QRT[:, cs], SB)
                pss = psrot.next()
                kb.mm(pss[:, 0:128], KZ[:, n, :], RV[:, n, :])
                kb.ts("dve", BX, pso[:, 128:256], XI, None, op0=ALU.mult)
                o = OT.next()
                kb.tt("dve", o, pso[:, 0:128], BX, ALU.add)
                kb.stt("dve", SF, SF, float(GC[h]), pss[:, 0:128], ALU.mult, ALU.add)
                kb.cp("act", SB, SF)
                _norm_gate_T(kb, psrot, IDENT, o, 128, SG[:, n, :], YR[:, cs], False, TMP)
            kb.dma("sp", nret_p[l, h], SF)
            for r in range(NS):
                ti = 16 + r
                kb.dma("sp", S0, sret[r, l, h])
                ps = psrot.next()
                kb.mm(ps[:, 0:1], RQ[0:1, ti, :], ONESF[0:1, 0:1])
                kb.ts("dve", COL[:, 0:1], ps[:, 0:1], SCALE, None, op0=ALU.mult)
                ps2 = psrot.next()
                kb.mm(ps2[0:1, 0:128], COL[:, 0:1], S0)
                kb.tt("dve", TA[0:1, :], RQ[0:1, ti, :], RK[0:1, ti, :], ALU.mult)
                kb.red("dve", SC1[:, 0:1], TA[0:1, :])
                kb.ts("dve", SC1[:, 0:1], SC1[:, 0:1], SCALE, None, op0=ALU.mult)
                kb.ts("dve", OS, ps2[0:1, 0:128], float(GAM[h]), None, op0=ALU.mult)
                kb.stt("dve", OS, RVS[r], SC1[0:1, 0:1], OS, ALU.mult, ALU.add)
                ps3 = psrot.next()
                kb.mm(ps3[:, 0:128], RK[0:1, ti, :], RVS[r])
                kb.stt("dve", TB, S0, float(GAM[h]), ps3[:, 0:128], ALU.mult, ALU.add)
                kb.dma("sp", nret_s[r, l, h], TB)
                _norm_gate_T(kb, psrot, IDENT, OS, 1, SGS[r], YR[:, T + r:T + r + 1], False, TMP)
            store_y(8 + h, YR)


def _mixer_gla(kb, L, g):
    l, W, UT = L["l"], L["W"], L["UT"]
    proj_tm, proj_fm, evac, store_y, psrot = L["proj_tm"], L["proj_fm"], L["evac"], L["store_y"], g["psrot"]
    cf, IDENT, ONESF, PS = g["cf"], g["IDENT"], g["ONESF"], g["PS"]
    ngla_p, ngla_s, sgla = g["ngla_p"], g["ngla_s"], g["sgla"]
    TRI64, SEL64 = cf("tri64"), cf("sel64")
    SCALE = 64.0 ** -0.5
    with kb.scope():
        GLRT = kb.sb([16, TT], BF16, "glrt")
        WA2 = kb.sb([16, 256], BF16, "wa2")
        kb.dma("pool", WA2, W["wa2"])
        BA = kb.sb([128, 256], F32, "ba")
        kb.dma("sp", BA, W["ba"].bc([128, 256]))
        proj_fm(OFF["glr"], 16, lambda c0, cn, ps: evac(GLRT[:, c0:c0 + cn], ps[0:16, 0:cn]))
        Bc = kb.sb([128, NTILE, 256], F32, "Bc")
        BLc = kb.sb([128, 16, 256], F32, "BLc")
        LG = Rot([kb.sb([128, 256], F32, f"lg{i}") for i in range(2)])
        for ti in range(NTILE):
            c0, m = L["tok_range"](ti)
            ps = psrot.next()
            kb.mm(ps[0:m, 0:256], GLRT[:, c0:c0 + m], WA2)
            lg = LG.next()
            kb.tt("dve", lg[0:m, :], ps[0:m, 0:256], BA[0:m, :], ALU.add)
            kb.act(lg[0:m, :], lg[0:m, :], AF.Exp, scale=-1.0)
            kb.act(lg[0:m, :], lg[0:m, :], AF.Ln, bias=1.0)
            kb.ts("dve", lg[0:m, :], lg[0:m, :], -1.0 / 16.0, None, op0=ALU.mult)
            if ti < 16:
                ps2 = psrot.next()
                kb.mm(ps2[:, 0:256], TRI64, lg)
                kb.cp("act", Bc[:, ti, :], ps2[:, 0:256])
                ps3 = psrot.next()
                kb.mm(ps3[:, 0:256], SEL64, Bc[:, ti, :])
                kb.cp("dve", BLc[:, ti, :], ps3[:, 0:256])
            else:
                kb.cp("dve", Bc[0:1, ti, :], lg[0:1, :])
        GQ = kb.sb([128, NTILE, 64], F32, "gq")
        GK = kb.sb([128, NTILE, 64], F32, "gk")
        GV = kb.sb([128, 16, 128], BF16, "gv")
        SGG = kb.sb([128, 16, 128], F32, "sgg")
        GVS = [kb.sb([1, 128], F32, f"gvs{r}") for r in range(NS)]
        SGS = [kb.sb([1, 128], F32, f"gsgs{r}") for r in range(NS)]
        YG = kb.sb([128, TT], BF16, "yg")
        SF = kb.sb([64, 128], F32, "gsf")
        SBs = Rot([kb.sb([64, 128], BF16, f"gsb{i}") for i in range(3)])
        EB = kb.sb([128, 64], F32, "eb")
        ENB = kb.sb([128, 64], F32, "enb")
        EZ = kb.sb([128, 64], F32, "ez")
        QB = kb.sb([128, 64], F32, "qbb")
        KBt = kb.sb([128, 64], F32, "kbb")
        KZA = kb.sb([128, 64], BF16, "gkza")
        KZB = kb.sb([128, 64], BF16, "gkzb")
        QBT = kb.sb([64, 128], BF16, "qbt")
        QBTA = kb.sb([64, 128], BF16, "qbta")
        QBTB = kb.sb([64, 128], BF16, "qbtb")
        kb.memset("dve", QBTA, 0.0)
        kb.memset("dve", QBTB, 0.0)
        MA = cf("tri64", lo=63, hi=64)
        MB = cf("tri64", lo=127, hi=128)
        KBT = kb.sb([64, 128], BF16, "kbt")
        EBL = kb.sb([64, 2], F32, "ebl")
        ATT = kb.sb([128, 128], BF16, "gatt")
        O = kb.sb([128, 128], F32, "go")
        TMP = dict(st=kb.sb([128, 4], F32, "gst"), sq=kb.sb([128, 128], F32, "gsq"), y=kb.sb([128, 128], F32, "gyn"))
        S0 = kb.sb([64, 128], F32, "gs0")
        SN = kb.sb([64, 128], F32, "gsn")
        COL = kb.sb([64, 2], F32, "gcol")
        SC1 = kb.sb([1, 4], F32, "gsc1")
        OS = kb.sb([1, 128], F32, "gos")
        R1 = kb.sb([1, 64], F32, "gr1")
        R2 = kb.sb([1, 64], F32, "gr2")

        def v_cons(ti, m, ps):
            if ti < 16:
                evac(GV[:, ti, :], ps[:, 0:128])
            else:
                kb.cp("dve", GVS[ti - 16], ps[0:1, 0:128])

        def g_cons(ti, m, ps):
            if ti < 16:
                kb.act(SGG[:, ti, :], ps[:, 0:128], AF.Silu)
            else:
                kb.act(SGS[ti - 16], ps[0:1, 0:128], AF.Silu)

        for h in range(4):
            hs = slice(h * 64, (h + 1) * 64)
            proj_tm(OFF["gq"] + h * 64, 64, lambda ti, m, ps: evac(GQ[0:m, ti, :], ps[0:m, 0:64], scale=SCALE))
            proj_tm(OFF["gk"] + h * 64, 64, lambda ti, m, ps: evac(GK[0:m, ti, :], ps[0:m, 0:64]))
            proj_tm(OFF["gv"] + h * 128, 128, v_cons)
            proj_tm(OFF["gg"] + h * 128, 128, g_cons)
            kb.memset("dve", SF, 0.0)
            sb_cur = SBs.next()
            kb.memset("dve", sb_cur, 0.0)
            for ti in range(16):
                cs = slice(ti * 128, (ti + 1) * 128)
                kb.act(EB, Bc[:, ti, hs], AF.Exp)
                kb.act(ENB, Bc[:, ti, hs], AF.Exp, scale=-1.0)
                kb.tt("dve", EZ, BLc[:, ti, hs], Bc[:, ti, hs], ALU.subtract)
                kb.act(EZ, EZ, AF.Exp)
                kb.tt("dve", QB, GQ[:, ti, :], EB, ALU.mult)
                kb.tt("pool", KBt, GK[:, ti, :], ENB, ALU.mult)
                kb.stt("dve", KZA, GK[:, ti, :], MA, EZ, ALU.mult, ALU.mult)
                kb.stt("dve", KZB, GK[:, ti, :], MB, EZ, ALU.mult, ALU.mult)
                ps = psrot.next()
                kb.tr(ps[0:64, 0:128], QB, IDENT)
                kb.tr(ps[0:64, 128:256], KBt, IDENT)
                kb.tr(ps[0:64, 256:384], BLc[:, ti, hs], IDENT)
                kb.cp("dve", QBT, ps[0:64, 0:128])
                kb.cp("dve", QBTA[:, 0:64], ps[0:64, 0:64])
                kb.cp("dve", QBTB[:, 64:128], ps[0:64, 64:128])
                kb.cp("act", KBT, ps[0:64, 128:256])
                kb.act(EBL[:, 0:1], ps[0:64, 256:257], AF.Exp)
                kb.act(EBL[:, 1:2], ps[0:64, 320:321], AF.Exp)
                psa = psrot.next()
                kb.mm(psa[:, 0:128], KBT, QBT)
                kb.tt("dve", ATT, psa[:, 0:128], TRI64, ALU.mult)
                pss = psrot.next()
                kb.mm(pss[0:64, 0:128], KZA, GV[:, ti, :])
                kb.stt("dve", SF, SF, EBL[:, 0:1], pss[0:64, 0:128], ALU.mult, ALU.add)
                sb_mid = SBs.next()
                kb.cp("act", sb_mid, SF)
                pso = psrot.next()
                kb.mm(pso[:, 0:128], ATT, GV[:, ti, :], start=True, stop=False)
                kb.mm(pso[:, 0:128], QBTA, sb_cur, start=False, stop=False)
                kb.mm(pso[:, 0:128], QBTB, sb_mid, start=False, stop=True)
                kb.cp("dve", O, pso[:, 0:128])
                pss2 = psrot.next()
                kb.mm(pss2[0:64, 0:128], KZB, GV[:, ti, :])
                kb.stt("dve", SF, SF, EBL[:, 1:2], pss2[0:64, 0:128], ALU.mult, ALU.add)
                sb_cur = SBs.next()
                kb.cp("act", sb_cur, SF)
                _norm_gate_T(kb, psrot, IDENT, O, 128, SGG[:, ti, :], YG[:, cs], True, TMP)
            kb.dma("sp", ngla_p[l, h], SF)
            for r in range(NS):
                ti = 16 + r
                kb.dma("sp", S0, sgla[r, l, h])
                kb.act(R1, Bc[0:1, ti, hs], AF.Exp)
                kb.tt("dve", R2, GQ[0:1, ti, :], R1, ALU.mult)
                ps = psrot.next()
                kb.mm(ps[0:64, 0:1], R2, ONESF[0:1, 0:1])
                kb.mm(ps[0:64, 1:2], R1, ONESF[0:1, 0:1])
                kb.cp("dve", COL, ps[0:64, 0:2])
                ps2 = psrot.next()
                kb.mm(ps2[0:1, 0:128], COL[:, 0:1], S0)
                kb.tt("dve", R2, GQ[0:1, ti, :], GK[0:1, ti, :], ALU.mult)
                kb.red("dve", SC1[:, 0:1], R2)
                kb.stt("dve", OS, GVS[r], SC1[0:1, 0:1], ps2[0:1, 0:128], ALU.mult, ALU.add)
                ps3 = psrot.next()
                kb.mm(ps3[0:64, 0:128], GK[0:1, ti, :], GVS[r])
                kb.stt("dve", SN, S0, COL[:, 1:2], ps3[0:64, 0:128], ALU.mult, ALU.add)
                kb.dma("sp", ngla_s[r, l, h], SN)
                _norm_gate_T(kb, psrot, IDENT, OS, 1, SGS[r], YG[:, T + r:T + r + 1], True, TMP)
            store_y(12 + h, YG)


def _phase_merge(kb, L, g):
    l, W, UT, winv = L["l"], L["W"], L["UT"], L["winv"]
    psrot, ytS, mgS = g["psrot"], g["ytS"], g["mgS"]
    slab_pool = L["slab_pool"]
    wbrv = W["wbr"].rr("n (cc p) d -> p (n cc) d", p=128)
    with kb.scope():
        YB = Rot([kb.sb([128, 16, BW], BF16, f"yb{i}") for i in range(2)])
        MBt = Rot([kb.sb([128, KC, BW], BF16, f"mbt{i}") for i in range(2)])
        BSL = Rot([kb.sb([128, 16, 128], BF16, f"bsl{i}") for i in range(2)])
        SGt = Rot([kb.sb([128, BW], F32, f"sgt{i}") for i in range(2)])
        TM = Rot([kb.sb([128, BW], F32, f"tmt{i}") for i in range(2)])
        ACC = Rot([kb.sb([128, BW], F32, f"acc{i}") for i in range(2)])
        for bi in range(NB):
            b0 = bi * BW
            yb = YB.next()
            kb.dma("sp", yb, ytS[:, :, b0:b0 + BW].rr("c p t -> p c t"))
            mb = MBt.next()
            for j in range(KC):
                gs = slab_pool.next()
                for n in range(4):
                    c0 = OFF["gates"] + n * D + j * 128
                    kb.dma("pool", gs[:, :, n * 128:(n + 1) * 128], winv[:, :, c0:c0 + 128])
                bs = BSL.next()
                kb.dma("pool", bs, wbrv[:, :, j * 128:(j + 1) * 128])
                acc = ACC.next()
                for n in range(4):
                    psg = psrot.next()
                    for k in range(KC):
                        kb.mm(psg[:, 0:BW], gs[:, k, n * 128:(n + 1) * 128], UT[:, k, b0:b0 + BW],
                              start=(k == 0), stop=(k == KC - 1))
                    psy = psrot.next()
                    for cc in range(4):
                        kb.mm(psy[:, 0:BW], bs[:, n * 4 + cc, :], yb[:, n * 4 + cc, :], start=(cc == 0), stop=(cc == 3))
                    sg = SGt.next()
                    kb.act(sg, psg[:, 0:BW], AF.Sigmoid)
                    if n == 0:
                        kb.tt("dve", acc, sg, psy[:, 0:BW], ALU.mult)
                    else:
                        tm = TM.next()
                        kb.tt("dve", tm, sg, psy[:, 0:BW], ALU.mult)
                        kb.tt("pool", acc, acc, tm, ALU.add)
                kb.cp("pool", mb[:, j, :], acc)
            kb.dma("sp", mgS[bi], mb)


def _segs(bi):
    b0 = bi * BW
    out = []
    a = 0
    while a < BW:
        gcol = b0 + a
        if gcol < T:
            b = min(BW, T - b0)
            out.append((a, b, 0))
            a = b
        else:
            out.append((a, a + 1, 1 + gcol - T))
            a += 1
    return out


def _phase_ffn(kb, L, g):
    l, W, last = L["l"], L["W"], L["last"]
    psrot, mgS, xaT, cf = g["psrot"], g["mgS"], g["xaT"], g["cf"]
    MOD, LNP, IDENT, ONESF = g["MOD"][l], g["LNP"][l], g["IDENT"], g["ONESF"]
    y_p, y_s = g["y_p"], g["y_s"]
    moe = (l % 2 == 1)
    wov = W["wo"].rr("(k p) n -> p k n", p=128)
    with kb.scope():
        AG1 = kb.sb([128, KC], F32, "ag1")
        AB1 = kb.sb([128, KC], F32, "ab1")
        kb.ts("dve", AG1, LNP[:, 0], ALPHA, None, op0=ALU.mult)
        kb.ts("dve", AB1, LNP[:, 1], ALPHA, None, op0=ALU.mult)
        HG = kb.sb([128, KC, 3], F32, "hg")
        HB = kb.sb([128, KC, 3], F32, "hb")
        for r in range(3):
            kb.tt("dve", HG[:, :, r], LNP[:, 0], MOD[:, 4, :, r], ALU.mult)
            kb.tt("dve", HB[:, :, r], LNP[:, 1], MOD[:, 4, :, r], ALU.mult)
            kb.tt("dve", HB[:, :, r], HB[:, :, r], MOD[:, 3, :, r], ALU.add)
        OG = kb.sb([128, KC], F32, "og")
        OB = kb.sb([128, KC], F32, "ob")
        kb.ts("dve", OG, LNP[:, 2], 1.0 if last else ALPHA, None, op0=ALU.mult)
        kb.ts("dve", OB, LNP[:, 3], 1.0 if last else ALPHA, None, op0=ALU.mult)
        MB = kb.sb([128, KC, BW], BF16, "mb")
        Y1 = kb.sb([128, KC, BW], F32, "y1")
        HV = kb.sb([128, KC, BW], BF16, "hv")
        C = kb.sb([128, KC, BW], F32, "cacc")
        HT = Rot([kb.sb([128, FCH, BW], BF16, f"ht{i}") for i in range(2)])
        WOS = Rot([kb.sb([128, KC, 128], BF16, f"wos{i}") for i in range(2)])
        GUS = Rot([kb.sb([128, KC, 256], BF16, f"gus{i}") for i in range(2)])
        DNS = Rot([kb.sb([128, FCH, 128], BF16, f"dns{i}") for i in range(2)])
        XA = Rot([kb.sb([128, BW], F32, f"xab{i}") for i in range(2)])
        SQ = Rot([kb.sb([128, BW], F32, f"sq{i}") for i in range(2)])
        HVF = Rot([kb.sb([128, BW], F32, f"hvf{i}") for i in range(1)])
        SGt = Rot([kb.sb([128, BW], F32, f"fsg{i}") for i in range(2)])
        TMt = Rot([kb.sb([128, BW], F32, f"ftm{i}") for i in range(1)])
        MEAN = kb.sb([128, BW], F32, "mean")
        M2 = kb.sb([128, BW], F32, "m2")
        RSTD = kb.sb([128, BW], F32, "rstd")
        if moe:
            WRF = kb.sb([128, KC, 8], F32, "wrf")
            kb.dma("sp", WRF, W["wr"].rr("(k p) x -> p k x", p=128))
            BR = kb.sb([8, 1], F32, "br")
            kb.dma("sp", BR, W["br"])
            LOG = kb.sb([8, BW], F32, "log")
            LT = kb.sb([128, 4, 8], F32, "lt")
            GT = kb.sb([128, 4, 8], F32, "gt")
            GTF = kb.sb([8, BW], F32, "gtf")
            GB = kb.sb([128, NEXP, BW], F32, "gb")
            TK = kb.sb([128, 8, 8], F32, "tk")
            EQb = kb.sb([128, 4, 8], F32, "eqb")
            L2b = kb.sb([128, 4, 8], F32, "l2b")
            SELb = kb.sb([128, 4, 8], F32, "selb")
            EXb = kb.sb([128, 4, 8], F32, "exb")
            kb.memset("dve", LT, 0.0)
        if last:
            YTM = Rot([kb.sb([128, D], F32, f"ytm{i}") for i in range(1)])
        SUBS = [(0, 128), (128, 128), (256, 128), (384, BW - 384)]

        def layer_norm(Y):
            ps1 = psrot.next()
            ps2 = psrot.next()
            for e in range(KC):
                kb.mm(ps1[:, 0:BW], ONESF, Y[:, e, :], start=(e == 0), stop=(e == KC - 1))
                sq = SQ.next()
                kb.act(sq, Y[:, e, :], AF.Square)
                kb.mm(ps2[:, 0:BW], ONESF, sq, start=(e == 0), stop=(e == KC - 1))
            kb.ts("dve", MEAN, ps1[:, 0:BW], 1.0 / D, None, op0=ALU.mult)
            kb.tt("dve", M2, MEAN, MEAN, ALU.mult)
            kb.stt("dve", RSTD, ps2[:, 0:BW], 1.0 / D, M2, ALU.mult, ALU.subtract)
            kb.act(RSTD, RSTD, AF.Ln, bias=LN_EPS)
            kb.act(RSTD, RSTD, AF.Exp, scale=-0.5)
            for e in range(KC):
                eng = "dve" if e % 2 == 0 else "pool"
                kb.tt(eng, Y[:, e, :], Y[:, e, :], MEAN, ALU.subtract)
                kb.tt(eng, Y[:, e, :], Y[:, e, :], RSTD, ALU.mult)

        import os
        FFL = int(os.environ.get("KDEV_FFN", "9"))
        for bi in range(NB):
            b0 = bi * BW
            segs = _segs(bi)
            kb.dma("sp", MB, mgS[bi])
            for e in range(KC):
                ws = WOS.next()
                kb.dma("pool", ws, wov[:, :, e * 128:(e + 1) * 128])
                ps = psrot.next()
                for k in range(KC):
                    kb.mm(ps[:, 0:BW], ws[:, k, :], MB[:, k, :], start=(k == 0), stop=(k == KC - 1))
                xa = XA.next()
                kb.dma("sp", xa, xaT[e, :, b0:b0 + BW])
                for (a, b, r) in segs:
                    kb.stt("dve", Y1[:, e, a:b], ps[:, a:b], MOD[:, 2, e, r:r + 1], xa[:, a:b], ALU.mult, ALU.add)
            if FFL < 2:
                continue
            layer_norm(Y1)
            if FFL < 3:
                continue
            if moe:
                psr = psrot.next()
            for e in range(KC):
                hvf = HVF.next()
                for (a, b, r) in segs:
                    kb.ts("dve", hvf[:, a:b], Y1[:, e, a:b], HG[:, e, r:r + 1], HB[:, e, r:r + 1], op0=ALU.mult, op1=ALU.add)
                if moe:
                    kb.mm(psr[0:8, 0:BW], WRF[:, e, :], hvf, start=(e == 0), stop=(e == KC - 1))
                kb.cp("act", HV[:, e, :], hvf)
                kb.ts("pool", Y1[:, e, :], Y1[:, e, :], AG1[:, e:e + 1], AB1[:, e:e + 1], op0=ALU.mult, op1=ALU.add)
            if moe:
                kb.ts("dve", LOG, psr[0:8, 0:BW], BR[:, 0:1], None, op0=ALU.add)
                pst = psrot.next()
                for si, (s0, sn) in enumerate(SUBS):
                    kb.tr(pst[0:sn, si * 8:(si + 1) * 8], LOG[:, s0:s0 + sn], IDENT[0:8, 0:8])
                for si, (s0, sn) in enumerate(SUBS):
                    kb.cp("dve", LT[0:sn, si, :], pst[0:sn, si * 8:(si + 1) * 8])
                m1, m2, t0, t1 = TK[:, 0, 0:4], TK[:, 1, 0:4], TK[:, 2, 0:4], TK[:, 3, 0:4]
                EQ, L2, SEL, EX = TK[:, 4:8, :], GT, TK[:, 4:8, :], GT
                kb.red("dve", m1, LT, op=ALU.max)
                for si in range(4):
                    kb.ts("dve", EQb[:, si, :], LT[:, si, :], m1[:, si:si + 1], None, op0=ALU.is_equal)
                kb.stt("dve", L2b, EQb, -1e30, LT, ALU.mult, ALU.add)
                kb.red("dve", m2, L2b, op=ALU.max)
                kb.tt("dve", t0, m2, m1, ALU.subtract)
                kb.act(t0, t0, AF.Exp)
                kb.ts("dve", t0, t0, 1.0, None, op0=ALU.add)
                kb.recip(t0, t0)
                kb.ts("dve", t1, m1, -1.0, None, op0=ALU.mult)
                for si in range(4):
                    kb.ts("dve", SELb[:, si, :], LT[:, si, :], m2[:, si:si + 1], None, op0=ALU.is_ge)
                    kb.act(EXb[:, si, :], LT[:, si, :], AF.Exp, bias=t1[:, si:si + 1], scale=1.0)
                    kb.stt("dve", GT[:, si, :], EXb[:, si, :], t0[:, si:si + 1], SELb[:, si, :], ALU.mult, ALU.mult)
                psg = psrot.next()
                for si, (s0, sn) in enumerate(SUBS):
                    kb.tr(psg[0:8, s0:s0 + sn], GT[0:sn, si, :], IDENT[0:sn, 0:sn])
                kb.cp("dve", GTF, psg[0:8, 0:BW])
                for x in range(NEXP):
                    psb = psrot.next()
                    kb.mm(psb[:, 0:BW], cf("sel8", rows=8, lo=x * 128, hi=(x + 1) * 128), GTF)
                    kb.cp("act", GB[:, x, :], psb[:, 0:BW])
            if FFL < 4:
                continue
            nx = NEXP if moe else 2
            for x in range(nx):
                if moe:
                    wg = W["eg"][x].rr("(k p) n -> p k n", p=128)
                    wu = W["eu"][x].rr("(k p) n -> p k n", p=128)
                    wd = W["ed"][x].rr("(f p) n -> p f n", p=128)
                else:
                    wg = W["fg"][:, x * DFE:(x + 1) * DFE].rr("(k p) n -> p k n", p=128)
                    wu = W["fu"][:, x * DFE:(x + 1) * DFE].rr("(k p) n -> p k n", p=128)
                    wd = W["fd"][x * DFE:(x + 1) * DFE, :].rr("(f p) n -> p f n", p=128)
                ht = HT.next()
                for f in range(FCH):
                    gu = GUS.next()
                    kb.dma("pool", gu[:, :, 0:128], wg[:, :, f * 128:(f + 1) * 128])
                    kb.dma("pool", gu[:, :, 128:256], wu[:, :, f * 128:(f + 1) * 128])
                    psg = psrot.next()
                    psu = psrot.next()
                    for k in range(KC):
                        kb.mm(psg[:, 0:BW], gu[:, k, 0:128], HV[:, k, :], start=(k == 0), stop=(k == KC - 1))
                    for k in range(KC):
                        kb.mm(psu[:, 0:BW], gu[:, k, 128:256], HV[:, k, :], start=(k == 0), stop=(k == KC - 1))
                    sg = SGt.next()
                    kb.act(sg, psg[:, 0:BW], AF.Silu)
                    if moe:
                        tm = TMt.next()
                        kb.tt("dve", tm, sg, psu[:, 0:BW], ALU.mult)
                        kb.tt("pool", ht[:, f, :], tm, GB[:, x, :], ALU.mult)
                    else:
                        kb.tt("dve", ht[:, f, :], sg, psu[:, 0:BW], ALU.mult)
                for e in range(KC):
                    ds = DNS.next()
                    kb.dma("pool", ds, wd[:, :, e * 128:(e + 1) * 128])
                    psd = psrot.next()
                    for f in range(FCH):
                        kb.mm(psd[:, 0:BW], ds[:, f, :], ht[:, f, :], start=(f == 0), stop=(f == FCH - 1))
                    if x == 0:
                        kb.cp("act", C[:, e, :], psd[:, 0:BW])
                    else:
                        kb.tt("dve", C[:, e, :], C[:, e, :], psd[:, 0:BW], ALU.add)
            if FFL < 5:
                continue
            for e in range(KC):
                for (a, b, r) in segs:
                    kb.stt("dve", C[:, e, a:b], C[:, e, a:b], MOD[:, 5, e, r:r + 1], Y1[:, e, a:b], ALU.mult, ALU.add)
            layer_norm(C)
            for e in range(KC):
                kb.ts("dve" if e % 2 == 0 else "pool", C[:, e, :], C[:, e, :], OG[:, e:e + 1], OB[:, e:e + 1],
                      op0=ALU.mult, op1=ALU.add)
            if FFL < 6:
                continue
            if not last:
                kb.dma("sp", xaT[:, :, b0:b0 + BW].rr("k p c -> p k c"), C, acc=True)
            else:
                for (s0, sn) in SUBS:
                    ytm = YTM.next()
                    for q4 in range(4):
                        ps = psrot.next()
                        for qq in range(4):
                            e = q4 * 4 + qq
                            kb.tr(ps[0:sn, qq * 128:(qq + 1) * 128], C[:, e, s0:s0 + sn], IDENT)
                        kb.cp("act" if q4 % 2 else "dve", ytm[0:sn, q4 * 512:(q4 + 1) * 512], ps[0:sn, :])
                    g0 = b0 + s0
                    npr = max(0, min(sn, T - g0))
                    if npr > 0:
                        kb.dma("sp", y_p[g0:g0 + npr, :], ytm[0:npr, :])
                    if npr < sn:
                        r0 = g0 + npr - T
                        kb.dma("sp", y_s[r0:r0 + (sn - npr), :], ytm[npr:sn, :])


_CACHE = {}


def core_inputs(z, c, wts, consts):
    cf_, rot_ = consts
    n_pool = z["cache_k"].shape[0]
    inp = dict(wts)
    inp["xp"] = z["x_prompt"][c]
    inp["xs"] = z["x_sample"][NS * c:NS * c + NS, 0, :]
    inp["crows"] = np.concatenate([z["c_prompt"][c:c + 1], z["c_sample"][NS * c:NS * c + NS]], 0)
    inp["ck"] = z["cache_k"].reshape(n_pool * 2 * 128, 512)
    inp["cv"] = z["cache_v"].reshape(n_pool * 2 * 128, 512)
    inp["clf"] = z["cache_logf"].reshape(n_pool * 2, 512)
    inp["pt"] = z["page_table"][NS * c:NS * c + NS].astype(np.int32)
    inp["spool"] = z["state_pool"][NS * c:NS * c + NS]
    inp["sret"] = z["state_ret"][NS * c:NS * c + NS]
    inp["sgla"] = z["state_gla"][NS * c:NS * c + NS]
    inp["constf"] = cf_
    inp["rotc"] = rot_
    return {k: np.ascontiguousarray(v) for k, v in inp.items()}


def kernel(**inputs):
    z = {k: np.asarray(v) for k, v in inputs.items()}
    if "nc" not in _CACHE:
        _CACHE["nc"] = build_program()
    nc = _CACHE["nc"]
    cf_, rot_, _, _ = make_consts()
    wts = weight_inputs(z)
    in_maps = [core_inputs(z, c, wts, (cf_, rot_)) for c in range(NCORE)]
    res = run_bass_kernel_spmd(nc, in_maps, core_ids=list(range(NCORE)))
    R = res.results
    B, SB = NCORE, NCORE * NS

    def cat(name):
        return np.stack([R[c][name] for c in range(NCORE)], 0)

    y_prompt = cat("y_p")
    y_sample = cat("y_s").reshape(SB, 1, D)
    nk_p = cat("nk_p").reshape(B, DEPTH, T, 4, 128)
    nv_p = cat("nv_p").reshape(B, DEPTH, T, 4, 128)
    nlf_p = cat("nlf_p")
    npool_p = cat("npool_p")
    nret_p = cat("nret_p")
    ngla_p = cat("ngla_p")
    nk_s = cat("nk_s").reshape(SB, DEPTH, 1, 4, 128)
    nv_s = cat("nv_s").reshape(SB, DEPTH, 1, 4, 128)
    nlf_s = cat("nlf_s").reshape(SB, DEPTH, 1, 4)
    npool_s = cat("npool_s").reshape(SB, DEPTH, 15, 512)
    nret_s = cat("nret_s").reshape(SB, DEPTH, 4, 128, 128)
    ngla_s = cat("ngla_s").reshape(SB, DEPTH, 4, 64, 128)
    return tuple(np.ascontiguousarray(a.astype(np.float32, copy=False)) for a in
                 (y_prompt, y_sample, nk_p, nv_p, nlf_p, npool_p, nret_p, ngla_p,
                  nk_s, nv_s, nlf_s, npool_s, nret_s, ngla_s))
```

```python
import math
from contextlib import ExitStack

import numpy as np
import concourse.bass as bass
import concourse.mybir as mybir
from concourse.bass_utils import run_bass_kernel_spmd

F32 = mybir.dt.float32
BF16 = mybir.dt.bfloat16
I32 = mybir.dt.int32
AF = mybir.ActivationFunctionType
ALU = mybir.AluOpType
AX = mybir.AxisListType

D = 2048
KC = 16
T = 2048
NS = 2
TT = T + NS
NCORE = 4
DEPTH = 2
PAST = 16384
NPAGE = 128
N_POOL = 1280
D_IN = 13844
OFF = dict(fq=0, fk=512, fv=1024, ff=1536, pin=1540, rq=2052, rk=2564, rv=3076, rg=3588,
           gq=4100, gk=4356, gv=4612, gg=5124, glr=5636, gates=5652)
NB = 5
BW = 410
ALPHA = (2.0 * DEPTH) ** 0.25
LN_EPS = 1e-5
COLT = [(0, 512), (512, 512), (1024, 512), (1536, 512), (2048, NS)]
NEXP = 8
DFE = 2816
FCH = DFE // 128


class Sem:
    _n = 0

    def __init__(self, h):
        self.h = h
        Sem._n += 1
        self.id = Sem._n
        self.total = 0


class Ev:
    __slots__ = ("sem", "val", "eng")

    def __init__(self, sem, val, eng):
        self.sem, self.val, self.eng = sem, val, eng


class Dep:
    __slots__ = ("w", "r", "dsem", "psum")

    def __init__(self):
        self.w = {}
        self.r = {}
        self.dsem = None
        self.psum = False


class View:
    __slots__ = ("ap", "dep", "sb")

    def __init__(self, ap, dep, sb):
        self.ap, self.dep, self.sb = ap, dep, sb

    def __getitem__(self, key):
        return View(self.ap[key], self.dep, self.sb)

    def rr(self, pat, **kw):
        return View(self.ap.rearrange(pat, **kw), self.dep, self.sb)

    def bc(self, shape):
        return View(self.ap.to_broadcast(shape), self.dep, self.sb)


ENG_ATTR = dict(pe="tensor", act="scalar", dve="vector", pool="gpsimd", sp="sync")
EPOCH = 20000
POOL_CAP = 4


class KB:
    def __init__(self, nc):
        self.nc = nc
        self.root = ExitStack()
        self.stack = [self.root]
        self.eng = {k: getattr(nc, v) for k, v in ENG_ATTR.items()}
        self.esem = {}
        self.tick = {}
        self.waited = {k: {} for k in ENG_ATTR}
        self.all_esems = []
        self.dsems = []
        self.final = {}
        self.ncnt = 0
        for k in ENG_ATTR:
            self._new_esem(k)
        self.dfree = []
        self.scope_ds = [[]]
        self.gsem = self._new_dsem()

    def _name(self, base):
        self.ncnt += 1
        return f"{base}_{self.ncnt}"

    def _new_esem(self, k):
        s = Sem(self.root.enter_context(self.nc.semaphore(self._name("es" + k))))
        self.esem[k] = s
        self.tick[k] = 0
        self.all_esems.append((k, s))

    def _new_dsem(self):
        if getattr(self, "dfree", None):
            s = self.dfree.pop()
        else:
            s = Sem(self.root.enter_context(self.nc.semaphore(self._name("ds"))))
            self.dsems.append(s)
        if hasattr(self, "scope_ds"):
            self.scope_ds[-1].append(s)
        return s

    def sb(self, shape, dt, name="t"):
        t = self.stack[-1].enter_context(self.nc.sbuf_tensor(self._name(name), list(shape), dt))
        return View(t[tuple(slice(None) for _ in shape)], Dep(), True)

    def ps(self, shape, dt, name="ps"):
        t = self.stack[-1].enter_context(self.nc.psum_tensor(self._name(name), list(shape), dt))
        d = Dep()
        d.psum = True
        return View(t[tuple(slice(None) for _ in shape)], d, True)

    def dram(self, name, shape, dt, kind):
        t = self.nc.dram_tensor(name, list(shape), dt, kind=kind)
        return View(t.ap(), Dep() if kind == "Internal" else None, False)

    def scope(self):
        kb = self

        class _S:
            def __enter__(s):
                es = ExitStack()
                kb.stack.append(es)
                kb.scope_ds.append([])
                return es

            def __exit__(s, *a):
                if getattr(kb, "finished", False):
                    return False
                kb.barrier()
                kb.stack.pop().close()
                kb.dfree.extend(kb.scope_ds.pop())
                return False

        return _S()

    def _wait(self, eng, need):
        e = self.eng[eng]
        wd = self.waited[eng]
        for ev in need.values():
            if wd.get(ev.sem.id, 0) < ev.val:
                e.wait_ge(ev.sem.h, ev.val)
                wd[ev.sem.id] = ev.val

    @staticmethod
    def _add(need, ev):
        o = need.get(ev.sem.id)
        if o is None or o.val < ev.val:
            need[ev.sem.id] = ev

    def op(self, eng, fn, R=(), W=()):
        need = {}
        wdeps = [v.dep for v in W if v is not None and v.dep is not None]
        rdeps = [v.dep for v in R if v is not None and v.dep is not None]
        for d in rdeps:
            for ev in d.w.values():
                self._add(need, ev)
            if d.psum:
                for ev in d.r.values():
                    if ev.eng != eng:
                        self._add(need, ev)
        for d in wdeps:
            for ev in d.w.values():
                if ev.eng != eng:
                    self._add(need, ev)
            for ev in d.r.values():
                if ev.eng != eng:
                    self._add(need, ev)
        self._wait(eng, need)
        if self.tick[eng] >= EPOCH:
            self._new_esem(eng)
        ins = fn(self.eng[eng])
        sem = self.esem[eng]
        self.tick[eng] += 1
        ins.then_inc(sem.h, 1)
        ev = Ev(sem, self.tick[eng], eng)
        for d in wdeps:
            d.w = {sem.id: ev}
            d.r = {}
        for d in rdeps:
            if d not in wdeps:
                d.r[sem.id] = ev
        return ins

    def dma(self, q, out, in_, acc=False, fn=None, final=False, R=()):
        need = {}
        od, idp = out.dep, in_.dep
        xdeps = [v.dep for v in R if v.dep is not None]
        for d in xdeps:
            for ev in d.w.values():
                self._add(need, ev)
        if out.sb and od is not None:
            owner = od
        elif in_.sb and idp is not None:
            owner = idp
        else:
            owner = None
        if owner is not None:
            if owner.dsem is None:
                owner.dsem = self._new_dsem()
            dsem = owner.dsem
        else:
            dsem = self.gsem
        if idp is not None:
            for ev in idp.w.values():
                self._add(need, ev)
        if od is not None:
            for ev in od.w.values():
                if acc or (ev.eng == "dma" and ev.sem is dsem):
                    continue
                self._add(need, ev)
            for ev in od.r.values():
                self._add(need, ev)
        if q == "pool":
            fifo = self.__dict__.setdefault("pool_fifo", [])
            if len(fifo) >= POOL_CAP:
                self._add(need, fifo[-POOL_CAP])
        self._wait(q, need)
        if fn is None:
            ins = self.eng[q].dma_start(out=out.ap, in_=in_.ap)
        else:
            ins = fn(self.eng[q])
        ins.then_inc(dsem.h, 16)
        dsem.total += 16
        ev = Ev(dsem, dsem.total, "dma")
        if q == "pool":
            self.pool_fifo.append(ev)
            if len(self.pool_fifo) > 64:
                del self.pool_fifo[:32]
        if od is not None:
            if acc:
                od.w[dsem.id] = ev
            else:
                od.w = {dsem.id: ev}
            od.r = {}
        if idp is not None:
            idp.r[dsem.id] = ev
        for d in xdeps:
            d.r[dsem.id] = ev
        if final or (od is None and not out.sb):
            self.final[dsem.id] = ev
        return ins

    def barrier(self):
        for k in ENG_ATTR:
            need = {}
            for k2, s in self.all_esems:
                if k2 != k:
                    v = self.tick[k2] if s is self.esem[k2] else EPOCH
                    if v > 0:
                        self._add(need, Ev(s, v, k2))
            for s in self.dsems:
                if s.total > 0:
                    self._add(need, Ev(s, s.total, "dma"))
            self._wait(k, need)

    def finish(self):
        self._wait("sp", dict(self.final))
        self.barrier()
        self.finished = True
        while len(self.stack) > 1:
            self.stack.pop().close()
        self.root.close()

    def mm(self, out, lhsT, rhs, start=True, stop=True):
        return self.op("pe", lambda e: e.matmul(out.ap, lhsT.ap, rhs.ap, start=start, stop=stop),
                       R=[lhsT, rhs], W=[out])

    def tr(self, out, in_, ident):
        return self.op("pe", lambda e: e.transpose(out.ap, in_.ap, ident.ap), R=[in_, ident], W=[out])

    def act(self, out, in_, func, bias=None, scale=None, accum=None, eng="act"):
        kw = {}
        R = [in_]
        if bias is not None:
            if isinstance(bias, View):
                kw["bias"] = bias.ap
                R.append(bias)
            else:
                kw["bias"] = bias
        if scale is not None:
            if isinstance(scale, View):
                kw["scale"] = scale.ap
                R.append(scale)
            else:
                kw["scale"] = scale
        W = [out]
        if accum is not None:
            kw["accum_out"] = accum.ap
            W.append(accum)
        return self.op(eng, lambda e: e.activation(out=out.ap, in_=in_.ap, func=func, **kw), R=R, W=W)

    def ts(self, eng, out, in0, s1, s2=None, op0=ALU.mult, op1=None, accum=None):
        R = [in0]
        a1 = s1
        if isinstance(s1, View):
            a1 = s1.ap
            R.append(s1)
        a2 = s2
        if isinstance(s2, View):
            a2 = s2.ap
            R.append(s2)
        kw = {}
        if op1 is not None:
            kw["op1"] = op1
        W = [out]
        if accum is not None:
            kw["accum_out"] = accum.ap
            W.append(accum)
        return self.op(eng, lambda e: e.tensor_scalar(out.ap, in0.ap, a1, a2, op0, **kw), R=R, W=W)

    def tt(self, eng, out, in0, in1, op):
        return self.op(eng, lambda e: e.tensor_tensor(out.ap, in0.ap, in1.ap, op), R=[in0, in1], W=[out])

    def stt(self, eng, out, in0, scalar, in1, op0, op1):
        R = [in0, in1]
        sc = scalar
        if isinstance(scalar, View):
            sc = scalar.ap
            R.append(scalar)
        return self.op(eng, lambda e: e.scalar_tensor_tensor(out.ap, in0.ap, sc, in1.ap, op0, op1), R=R, W=[out])

    def cp(self, eng, out, in_):
        if eng == "act":
            return self.op("act", lambda e: e.copy(out.ap, in_.ap), R=[in_], W=[out])
        return self.op(eng, lambda e: e.tensor_copy(out.ap, in_.ap), R=[in_], W=[out])

    def red(self, eng, out, in_, op=ALU.add, axis=None):
        ax = axis if axis is not None else AX.X
        return self.op(eng, lambda e: e.tensor_reduce(out.ap, in_.ap, ax, op), R=[in_], W=[out])

    def memset(self, eng, out, val):
        return self.op(eng, lambda e: e.memset(out.ap, val), R=[], W=[out])

    def recip(self, out, in_):
        return self.op("dve", lambda e: e.reciprocal(out.ap, in_.ap), R=[in_], W=[out])


def _load_rows_T(kb, psrot, IDENT, dst, src_rows, n):
    tmp = kb.sb([128, 128], F32, "lrt")
    kb.dma("sp", tmp[0:n, :], src_rows)
    ps = psrot.next()
    kb.tr(ps[:, 0:n], tmp[0:n, :], IDENT[0:n, 0:n])
    kb.cp("dve", dst, ps[:, 0:n])


class Rot:
    def __init__(self, items):
        self.items = items
        self.i = 0

    def next(self):
        v = self.items[self.i % len(self.items)]
        self.i += 1
        return v


CF = {}
_o = 0
for _n, _w in [("ident", 128), ("tri", 128), ("ones", 128), ("sufx", 128), ("tri64", 128), ("sel64", 128),
               ("dmat", 512), ("xi", 4), ("zeta", 4), ("pfix", 64), ("bd", 512), ("sel8", 1024)]:
    CF[_n] = (_o, _w)
    _o += _w
NCF = _o
NTILE = 18


def make_consts():
    c = np.zeros((128, NCF), np.float32)
    a = np.arange(128)

    def put(name, m):
        o, w = CF[name]
        c[: m.shape[0], o:o + m.shape[1]] = m

    put("ident", np.eye(128, dtype=np.float32))
    put("tri", (a[:, None] <= a[None, :]).astype(np.float32))
    put("ones", np.ones((128, 128), np.float32))
    put("sufx", (a[:, None] > a[None, :]).astype(np.float32))
    same = (a[:, None] // 64) == (a[None, :] // 64)
    put("tri64", (same & (a[:, None] <= a[None, :])).astype(np.float32))
    put("sel64", (a[:, None] == (a[None, :] // 64) * 64 + 63).astype(np.float32))
    logg = np.log1p(-np.exp2(-5.0 - np.arange(4, dtype=np.float32))).astype(np.float32)
    n = np.arange(128, dtype=np.float32)
    diff = n[None, :] - n[:, None]
    dm = np.zeros((128, 512), np.float32)
    for h in range(4):
        dm[:, h * 128:(h + 1) * 128] = np.where(diff >= 0, np.exp(np.where(diff >= 0, diff, 0.0) * logg[h]), 0.0)
    put("dmat", dm)
    put("xi", np.exp((n[:, None] + 1.0) * logg[None, :]).astype(np.float32))
    put("zeta", np.exp((127.0 - n)[:, None] * logg[None, :]).astype(np.float32))
    pf = np.ones((128, 64), np.float32)
    for g, w in enumerate((2, 4, 8, 16)):
        t = np.arange(16)
        pf[:, g * 16:(g + 1) * 16] = (np.float32(w) / np.minimum(w, t + 1).astype(np.float32))[None, :]
    put("pfix", pf)
    bd = np.zeros((4, 512), np.float32)
    for h in range(4):
        bd[h, h * 128:(h + 1) * 128] = 1.0
    put("bd", bd)
    s8 = np.zeros((8, 1024), np.float32)
    for e in range(8):
        s8[e, e * 128:(e + 1) * 128] = 1.0
    put("sel8", s8)
    half = 64
    freqs = (np.float32(10000.0) ** (-np.arange(half, dtype=np.float32) / np.float32(half))).astype(np.float32)
    rot = np.zeros((128, 2, NTILE, 128), np.float32)
    for ti in range(NTILE):
        pos = (ti * 128 + a).astype(np.float32) if ti < 16 else np.full(128, PAST, np.float32)
        ang = (pos[:, None] * freqs[None, :]).astype(np.float32).astype(np.float64)
        cs, sn = np.cos(ang).astype(np.float32), np.sin(ang).astype(np.float32)
        rot[:, 0, ti, :64] = cs
        rot[:, 0, ti, 64:] = cs
        rot[:, 1, ti, :64] = -sn
        rot[:, 1, ti, 64:] = sn
    gam = np.exp(logg.astype(np.float64))
    gc = np.exp(128.0 * logg.astype(np.float64))
    return c, rot.reshape(128, 2 * NTILE * 128), [float(x) for x in gam], [float(x) for x in gc]


def build_program(depth=DEPTH, n_pool=N_POOL, stop_after=None):
    nc = bass.Bass("TRN2", target_bir_lowering=False)
    kb = KB(nc)
    _, _, GAM, GC = make_consts()

    def ein(name, shape, dt=F32):
        return kb.dram(name, shape, dt, "ExternalInput")

    def eout(name, shape):
        return kb.dram(name, shape, F32, "ExternalOutput")

    xp = ein("xp", [T, D])
    xs = ein("xs", [NS, D])
    crows = ein("crows", [3, D])
    ck = ein("ck", [n_pool * 2 * 128, 512])
    cv = ein("cv", [n_pool * 2 * 128, 512])
    clf = ein("clf", [n_pool * 2, 512])
    pt = ein("pt", [NS, NPAGE], I32)
    spool = ein("spool", [NS, DEPTH, 15, 512])
    sret = ein("sret", [NS, DEPTH, 4, 128, 128])
    sgla = ein("sgla", [NS, DEPTH, 4, 64, 128])
    constf = ein("constf", [128, NCF])
    rotc = ein("rotc", [128, 2 * NTILE * 128])
    Wt = []
    for l in range(depth):
        w = dict(
            ada=ein(f"w_ada{l}", [D, 6 * D]), bada=ein(f"b_ada{l}", [96, 128]),
            win=ein(f"w_in{l}", [D, D_IN]), bfox=ein(f"b_fox{l}", [1, 4]),
            wa2=ein(f"w_a2{l}", [16, 256]), ba=ein(f"b_a{l}", [1, 256]),
            wmap=ein(f"w_map{l}", [4, 128, 128]), pscale=ein(f"pscale{l}", [4, 128]),
            wbr=ein(f"w_br{l}", [4, 512, D]), wo=ein(f"w_o{l}", [D, D]),
            ln=ein(f"ln{l}", [64, 128]),
        )
        if l % 2 == 0:
            w["fg"] = ein(f"w_fg{l}", [D, 2 * DFE])
            w["fu"] = ein(f"w_fu{l}", [D, 2 * DFE])
            w["fd"] = ein(f"w_fd{l}", [2 * DFE, D])
        else:
            w["wr"] = ein(f"w_r{l}", [D, 8])
            w["br"] = ein(f"b_r{l}", [8, 1])
            w["eg"] = ein(f"w_eg{l}", [NEXP, D, DFE])
            w["eu"] = ein(f"w_eu{l}", [NEXP, D, DFE])
            w["ed"] = ein(f"w_ed{l}", [NEXP, DFE, D])
        Wt.append(w)

    y_p = eout("y_p", [T, D])
    y_s = eout("y_s", [NS, D])
    nk_p = eout("nk_p", [DEPTH, T, 512])
    nv_p = eout("nv_p", [DEPTH, T, 512])
    nlf_p = eout("nlf_p", [DEPTH, T, 4])
    npool_p = eout("npool_p", [DEPTH, 15, 512])
    nret_p = eout("nret_p", [DEPTH, 4, 128, 128])
    ngla_p = eout("ngla_p", [DEPTH, 4, 64, 128])
    nk_s = eout("nk_s", [NS, DEPTH, 512])
    nv_s = eout("nv_s", [NS, DEPTH, 512])
    nlf_s = eout("nlf_s", [NS, DEPTH, 4])
    npool_s = eout("npool_s", [NS, DEPTH, 15, 512])
    nret_s = eout("nret_s", [NS, DEPTH, 4, 128, 128])
    ngla_s = eout("ngla_s", [NS, DEPTH, 4, 64, 128])

    xaT = kb.dram("xaT", [KC, 128, TT], F32, "Internal")
    ytS = kb.dram("ytS", [16, 128, TT], BF16, "Internal")
    mgS = kb.dram("mgS", [NB, 128, KC, BW], BF16, "Internal")

    CFt = kb.sb([128, NCF], F32, "constf")
    kb.dma("sp", CFt, constf)

    def cf(name, rows=128, lo=0, hi=None):
        o, w = CF[name]
        hi = w if hi is None else hi
        return CFt[0:rows, o + lo:o + hi]

    IDENT = cf("ident")
    ONESF = cf("ones")
    TRI = cf("tri")
    ONESB = kb.sb([128, 128], BF16, "onesb")
    kb.memset("dve", ONESB, 1.0)
    PS = [kb.ps([128, 512], F32, f"bank{i}") for i in range(8)]
    psrot = Rot(PS)
    MOD = [kb.sb([128, 6, KC, 3], F32, f"mod{l}") for l in range(depth)]
    LNP = [kb.sb([128, 4, KC], F32, f"lnp{l}") for l in range(depth)]

    def load_rows_T(dst, src_rows, n):
        return _load_rows_T(kb, psrot, IDENT, dst, src_rows, n)

    def _unused(dst, src_rows, n):
        tmp = kb.sb([128, 128], F32, "lrt")
        kb.dma("sp", tmp[0:n, :], src_rows)
        ps = psrot.next()
        kb.tr(ps[:, 0:n], tmp[0:n, :], IDENT[0:n, 0:n])
        kb.cp("dve", dst, ps[:, 0:n])

    with kb.scope():
        CR = kb.sb([3, D], F32, "crows")
        kb.dma("sp", CR, crows)
        SCR = kb.sb([3, D], F32, "scr")
        kb.act(SCR, CR, AF.Silu)
        scT = kb.sb([128, KC, 3], BF16, "scT")
        ps = psrot.next()
        for k in range(KC):
            kb.tr(ps[:, k * 3:(k + 1) * 3], SCR[:, k * 128:(k + 1) * 128], IDENT[0:3, 0:3])
        kb.cp("dve", scT.rr("p k r -> p (k r)"), ps[:, 0:KC * 3])
        slabs = Rot([kb.sb([128, KC, 1024], BF16, f"adaslab{i}") for i in range(2)])
        for l in range(depth):
            BAD = kb.sb([128, 96], F32, "bada")
            load_rows_T(BAD, Wt[l]["bada"], 96)
            load_rows_T(LNP[l].rr("p a k -> p (a k)"), Wt[l]["ln"], 64)
            wv = Wt[l]["ada"].rr("(k p) n -> p k n", p=128)
            for g in range(12):
                sl = slabs.next()
                kb.dma("pool", sl, wv[:, :, g * 1024:(g + 1) * 1024])
                ps = psrot.next()
                for j in range(8):
                    for k in range(KC):
                        kb.mm(ps[:, j * 4:j * 4 + 3], sl[:, k, j * 128:(j + 1) * 128], scT[:, k, :],
                              start=(k == 0), stop=(k == KC - 1))
                part, kk0 = (g * 8) // 16, (g * 8) % 16
                for r in range(3):
                    kb.tt("dve", MOD[l][:, part, kk0:kk0 + 8, r],
                          ps[:, 0:32].rr("p (j q) -> p j q", q=4)[:, :, r], BAD[:, g * 8:(g + 1) * 8], ALU.add)
            for part in (1, 4):
                kb.ts("dve", MOD[l][:, part], MOD[l][:, part], 1.0, None, op0=ALU.add)
    if stop_after == "p0":
        dbg = eout("dbg", [128, 6 * KC * 3])
        kb.dma("sp", dbg, MOD[0].rr("p a k r -> p (a k r)"))
        kb.finish()
        return nc
    return _build_layers(kb, locals())


def weight_inputs(z, depth=DEPTH):
    w = {}
    for l in range(depth):
        w[f"w_ada{l}"] = z["w_ada"][l]
        w[f"b_ada{l}"] = z["b_ada"][l].reshape(96, 128)
        w[f"w_in{l}"] = z["w_in"][l]
        w[f"b_fox{l}"] = z["b_fox_f"][l].reshape(1, 4)
        w[f"w_a2{l}"] = z["w_gla_a2"][l]
        w[f"b_a{l}"] = z["b_gla_a"][l].reshape(1, 256)
        w[f"w_map{l}"] = z["w_pool_map"][l]
        w[f"pscale{l}"] = z["pool_scale"][l].reshape(4, 128)
        w[f"w_br{l}"] = z["w_branch"][l]
        w[f"w_o{l}"] = z["w_o"][l]
        w[f"ln{l}"] = np.concatenate([z["ln1_g"][l], z["ln1_b"][l], z["ln2_g"][l], z["ln2_b"][l]]).reshape(64, 128)
        j = l // 2
        if l % 2 == 0:
            w[f"w_fg{l}"] = z["w_ff_gate"][j]
            w[f"w_fu{l}"] = z["w_ff_up"][j]
            w[f"w_fd{l}"] = z["w_ff_down"][j]
        else:
            w[f"w_r{l}"] = z["w_router"][j]
            w[f"b_r{l}"] = z["b_router"][j].reshape(8, 1)
            w[f"w_eg{l}"] = z["w_ex_gate"][j]
            w[f"w_eu{l}"] = z["w_ex_up"][j]
            w[f"w_ed{l}"] = z["w_ex_down"][j]
    return w


def _build_layers(kb, g):
    nc = g["nc"]
    depth, n_pool, stop_after = g["depth"], g["n_pool"], g["stop_after"]
    Wt, MOD, LNP, PS, psrot, cf = g["Wt"], g["MOD"], g["LNP"], g["PS"], g["psrot"], g["cf"]
    IDENT, ONESF, TRI, ONESB = g["IDENT"], g["ONESF"], g["TRI"], g["ONESB"]
    GAM, GC = g["GAM"], g["GC"]
    xp, xs, xaT, ytS, mgS = g["xp"], g["xs"], g["xaT"], g["ytS"], g["mgS"]
    eout = g["eout"]
    evq = Rot(["dve", "act"])

    def evac(out, in_, scale=None):
        e = evq.next()
        if scale is None:
            kb.cp(e, out, in_)
        elif e == "dve":
            kb.ts("dve", out, in_, float(scale), None, op0=ALU.mult)
        else:
            kb.act(out, in_, AF.Copy, scale=float(scale))

    def tok_range(ti):
        if ti < 16:
            return ti * 128, 128
        return T + (ti - 16), 1

    for l in range(depth):
        W = Wt[l]
        last = (l == depth - 1)
        winv = W["win"].rr("(k p) n -> p k n", p=128)
        with kb.scope():
            UT = kb.sb([128, KC, TT], BF16, "UT")
            with kb.scope():
                if l == 0:
                    xts = Rot([kb.sb([128, D], F32, f"xt{i}") for i in range(2)])
                    xas = Rot([kb.sb([128, KC, 128], F32, f"xa{i}") for i in range(2)])
                    import os
                    for ti in [int(v) for v in os.environ.get('KDEV_TILES', ','.join(str(i) for i in range(17))).split(',') if v != '']:
                        xt = xts.next()
                        xa = xas.next()
                        m = 128 if ti < 16 else NS
                        c0 = ti * 128
                        kb.dma("sp", xt[0:m, :], xp[ti * 128:(ti + 1) * 128, :] if ti < 16 else xs)
                        for q4 in range(4):
                            ps = psrot.next()
                            for qq in range(4):
                                e = q4 * 4 + qq
                                kb.tr(ps[:, qq * 128:qq * 128 + m], xt[0:m, e * 128:(e + 1) * 128], IDENT[0:m, 0:m])
                            for qq in range(4):
                                e = q4 * 4 + qq
                                if os.environ.get("KDEV_NOACT"):
                                    continue
                                if os.environ.get("KDEV_ACTF"):
                                    kb.act(UT[:, e, c0:c0 + 128], ps[:, qq * 128:(qq + 1) * 128], AF.Identity, bias=0.5, scale=2.0)
                                elif ti < 16:
                                    kb.act(UT[:, e, c0:c0 + 128], ps[:, qq * 128:(qq + 1) * 128], AF.Identity,
                                           bias=MOD[l][:, 0, e, 0:1], scale=MOD[l][:, 1, e, 0:1])
                                else:
                                    for r in range(NS):
                                        kb.act(UT[:, e, T + r:T + r + 1], ps[:, qq * 128 + r:qq * 128 + r + 1], AF.Identity,
                                               bias=MOD[l][:, 0, e, 1 + r:2 + r], scale=MOD[l][:, 1, e, 1 + r:2 + r])
                            kb.ts("dve", xa[:, q4 * 4:(q4 + 1) * 4, 0:m],
                                  ps.rr("p (q c) -> p q c", c=128)[:, :, 0:m], ALPHA, None, op0=ALU.mult)
                        if not os.environ.get("KDEV_NOXA"):
                            kb.dma("sp", xaT[:, :, c0:c0 + m].rr("k p c -> p k c"), xa[:, :, 0:m], acc=True)
                else:
                    SCA = kb.sb([128, KC, 3], F32, "sca")
                    kb.ts("dve", SCA, MOD[l][:, 1], 1.0 / ALPHA, None, op0=ALU.mult)
                    xcs = Rot([kb.sb([128, TT], F32, f"xc{i}") for i in range(2)])
                    for e in range(KC):
                        xc = xcs.next()
                        kb.dma("sp", xc, xaT[e])
                        kb.act(UT[:, e, 0:T], xc[:, 0:T], AF.Identity, bias=MOD[l][:, 0, e, 0:1], scale=SCA[:, e, 0:1])
                        for r in range(NS):
                            kb.act(UT[:, e, T + r:T + r + 1], xc[:, T + r:T + r + 1], AF.Identity,
                                   bias=MOD[l][:, 0, e, 1 + r:2 + r], scale=SCA[:, e, 1 + r:2 + r])
            if stop_after == "pA":
                dbg = eout("dbg", [KC, 128, TT])
                DB = kb.sb([128, TT], F32, "dbgt")
                for e in range(KC):
                    kb.cp("dve", DB, UT[:, e, :])
                    kb.dma("sp", dbg[e], DB)
                kb.finish()
                return nc

            mix_scope = kb.scope()
            mix_scope.__enter__()
            slab_pool = Rot([kb.sb([128, KC, 512], BF16, f"slab{i}") for i in range(2)])

            def load_slab(col0, ncols):
                sl = slab_pool.next()
                kb.dma("pool", sl[:, :, 0:ncols], winv[:, :, col0:col0 + ncols])
                return sl

            def proj_tm(col0, ncols, consume, tiles=range(NTILE)):
                sl = load_slab(col0, ncols)
                for ti in tiles:
                    c0, m = tok_range(ti)
                    ps = psrot.next()
                    for k in range(KC):
                        kb.mm(ps[0:m, 0:ncols], UT[:, k, c0:c0 + m], sl[:, k, 0:ncols], start=(k == 0), stop=(k == KC - 1))
                    consume(ti, m, ps)

            def proj_fm(col0, ncols, consume):
                sl = load_slab(col0, ncols)
                for (c0, cn) in COLT:
                    ps = psrot.next()
                    for k in range(KC):
                        kb.mm(ps[0:ncols, 0:cn], sl[:, k, 0:ncols], UT[:, k, c0:c0 + cn], start=(k == 0), stop=(k == KC - 1))
                    consume(c0, cn, ps)

            def store_y(chunk, Y):
                kb.dma("sp", ytS[chunk], Y, acc=True)

            _mixer_fox(kb, locals(), g)
            if stop_after == "fox":
                dbgy = kb.dram("dbg_y", [16, 128, TT], BF16, "ExternalOutput")
                kb.dma("sp", dbgy, ytS)
                kb.finish()
                return nc
            _mixer_pool(kb, locals(), g)
            if stop_after == "pool":
                dbgy = kb.dram("dbg_y", [16, 128, TT], BF16, "ExternalOutput")
                kb.dma("sp", dbgy, ytS)
                kb.finish()
                return nc
            _mixer_ret(kb, locals(), g)
            if stop_after == "ret":
                dbgy = kb.dram("dbg_y", [16, 128, TT], BF16, "ExternalOutput")
                kb.dma("sp", dbgy, ytS)
                kb.finish()
                return nc
            _mixer_gla(kb, locals(), g)
            if stop_after == "gla":
                dbgy = kb.dram("dbg_y", [16, 128, TT], BF16, "ExternalOutput")
                kb.dma("sp", dbgy, ytS)
                kb.finish()
                return nc
            mix_scope.__exit__(None, None, None)
            _phase_merge(kb, locals(), g)
            if stop_after == "merge":
                dbgm = kb.dram("dbg_m", [NB, 128, KC, BW], BF16, "ExternalOutput")
                kb.dma("sp", dbgm, mgS)
                kb.finish()
                return nc
        _phase_ffn(kb, locals(), g)
    kb.finish()
    return nc


def _mixer_fox(kb, L, g):
    l, W, UT = L["l"], L["W"], L["UT"]
    proj_tm, proj_fm, evac, store_y, psrot = L["proj_tm"], L["proj_fm"], L["evac"], L["store_y"], g["psrot"]
    cf, IDENT, ONESF, TRI, ONESB, PS = g["cf"], g["IDENT"], g["ONESF"], g["TRI"], g["ONESB"], g["PS"]
    nk_p, nv_p, nlf_p, nk_s, nv_s, nlf_s = g["nk_p"], g["nv_p"], g["nlf_p"], g["nk_s"], g["nv_s"], g["nlf_s"]
    ck, cv, clf, pt = g["ck"], g["cv"], g["clf"], g["pt"]
    SCALE = 128.0 ** -0.5
    with kb.scope():
        KNEW = [kb.sb([1, 512], F32, f"knew{r}") for r in range(NS)]
        VNEW = [kb.sb([1, 512], F32, f"vnew{r}") for r in range(NS)]
        QROW = [kb.sb([1, 512], F32, f"qrow{r}") for r in range(NS)]
        LFR = kb.sb([128, NTILE, 4], F32, "lfraw")
        kb.memset("dve", LFR, 0.0)
        LF = kb.sb([128, NTILE, 4], F32, "lf")
        YF = [kb.sb([128, TT], BF16, f"yf{h}") for h in range(4)]
        inner = kb.scope()
        inner.__enter__()
        VT = kb.sb([128, 16, 512], BF16, "VT")
        stg = Rot([kb.sb([128, 512], F32, f"stg{i}") for i in range(2)])

        def k_cons(ti, m, ps):
            if ti < 16:
                s = stg.next()
                evac(s[0:m, :], ps[0:m, 0:512])
                kb.dma("sp", nk_p[l, ti * 128:(ti + 1) * 128, :], s[0:m, :])
            else:
                r = ti - 16
                kb.cp("dve", KNEW[r], ps[0:1, 0:512])
                kb.dma("sp", nk_s[r, l:l + 1, :], KNEW[r])

        def v_cons(ti, m, ps):
            if ti < 16:
                s = stg.next()
                evac(s[0:m, :], ps[0:m, 0:512])
                kb.dma("sp", nv_p[l, ti * 128:(ti + 1) * 128, :], s[0:m, :])
                kb.cp("act", VT[:, ti, :], ps[:, 0:512])
            else:
                r = ti - 16
                kb.cp("dve", VNEW[r], ps[0:1, 0:512])
                kb.dma("sp", nv_s[r, l:l + 1, :], VNEW[r])

        proj_tm(OFF["fk"], 512, k_cons)
        proj_tm(OFF["fv"], 512, v_cons)
        proj_tm(OFF["ff"], 4, lambda ti, m, ps: kb.cp("dve", LFR[0:m, ti, :], ps[0:m, 0:4]))
        proj_tm(OFF["fq"], 512, lambda ti, m, ps: kb.ts("dve", QROW[ti - 16], ps[0:1, 0:512], SCALE, None, op0=ALU.mult),
                tiles=range(16, NTILE))

        BF4 = kb.sb([128, 4], F32, "bf4")
        kb.dma("sp", BF4, W["bfox"].bc([128, 4]))
        kb.tt("dve", LF, LFR, BF4.rr("p (o h) -> p o h", o=1).bc([128, NTILE, 4]), ALU.add)
        kb.act(LF, LF, AF.Exp, scale=-1.0)
        kb.act(LF, LF, AF.Ln, bias=1.0)
        kb.ts("dve", LF, LF, -1.0, None, op0=ALU.mult)
        kb.dma("sp", nlf_p[l].rr("(i p) h -> p i h", p=128), LF[:, 0:16, :])
        for r in range(NS):
            kb.dma("sp", nlf_s[r, l:l + 1, :], LF[0:1, 16 + r, :])
        LF2 = LF[:, 0:16, :].rr("p i h -> p (i h)")
        psA = psrot.next()
        kb.mm(psA[:, 0:64], TRI, LF2)
        psB = psrot.next()
        kb.mm(psB[:, 0:64], ONESF, LF2)
        TOT = kb.sb([128, 16, 4], F32, "tot")
        kb.cp("dve", TOT.rr("p i h -> p (i h)"), psB[:, 0:64])
        CI = kb.sb([128, 16, 4], F32, "ci")
        kb.cp("dve", CI[:, 0, :], TOT[:, 0, :])
        for i in range(1, 16):
            kb.tt("dve", CI[:, i, :], CI[:, i - 1, :], TOT[:, i, :], ALU.add)
        G = kb.sb([128, 16, 4], F32, "G")
        kb.tt("dve", G.rr("p i h -> p (i h)"), psA[:, 0:64], CI.rr("p i h -> p (i h)"), ALU.add)
        kb.tt("dve", G, G, TOT, ALU.subtract)

        QT = kb.sb([128, TT], BF16, "QT")
        KT = kb.sb([128, TT], BF16, "KT")
        BIAS = kb.sb([128, 16, 16], F32, "bias")
        pts = Rot([kb.sb([128, 128], BF16, f"pt{i}") for i in range(4)])
        RL = kb.sb([128, 128], F32, "rl")
        sbanks = Rot([PS[0], PS[1]])
        obanks = Rot([PS[2], PS[3]])
        lbanks = Rot([PS[4], PS[5]])
        import os
        FOXLVL = int(os.environ.get("KDEV_FOX", "3"))
        for h in range(4 if FOXLVL >= 2 else 0):
            proj_fm(OFF["fq"] + h * 128, 128, lambda c0, cn, ps: evac(QT[:, c0:c0 + cn], ps[:, 0:cn], scale=SCALE))
            proj_fm(OFF["fk"] + h * 128, 128, lambda c0, cn, ps: evac(KT[:, c0:c0 + cn], ps[:, 0:cn]))
            for i in range(16):
                kb.ts("dve", BIAS[:, i, :], G[:, :, h], -1.0, CI[:, i, h:h + 1], op0=ALU.mult, op1=ALU.add)
            for i in range(16):
                ob = obanks.next()
                lb = lbanks.next()
                for j0 in range(0, i + 1, 4):
                    js = list(range(j0, min(j0 + 4, i + 1)))
                    sb = sbanks.next()
                    for jj, j in enumerate(js):
                        kb.mm(sb[:, jj * 128:(jj + 1) * 128], KT[:, j * 128:(j + 1) * 128], QT[:, i * 128:(i + 1) * 128])
                    for jj, j in enumerate(js):
                        p_ = pts.next()
                        kb.act(p_, sb[:, jj * 128:(jj + 1) * 128], AF.Exp, bias=BIAS[:, i, j:j + 1], scale=1.0)
                        if j == i:
                            kb.tt("pool", p_, p_, TRI, ALU.mult)
                        kb.mm(ob[:, 0:128], VT[:, j, h * 128:(h + 1) * 128], p_, start=(j == 0), stop=(j == i))
                        kb.mm(lb[:, 0:128], ONESB, p_, start=(j == 0), stop=(j == i))
                kb.recip(RL, lb[:, 0:128])
                kb.tt("dve", YF[h][:, i * 128:(i + 1) * 128], ob[:, 0:128], RL, ALU.mult)

        inner.__exit__(None, None, None)
        if FOXLVL >= 3:
            _fox_sample(kb, L, g, dict(KNEW=KNEW, VNEW=VNEW, QROW=QROW, LF=LF, YF=YF))
        for h in range(4):
            store_y(h, YF[h])


def _fox_sample(kb, L, g, S):
    l, W = L["l"], L["W"]
    psrot, cf, IDENT, ONESF, PS = g["psrot"], g["cf"], g["IDENT"], g["ONESF"], g["PS"]
    ck, cv, clf, pt, n_pool = g["ck"], g["cv"], g["clf"], g["pt"], g["n_pool"]
    KNEW, VNEW, QROW, LF, YF = S["KNEW"], S["VNEW"], S["QROW"], S["LF"], S["YF"]
    SUFX = cf("sufx")
    SCALE = 128.0 ** -0.5
    with kb.scope():
        IOTA = kb.sb([128, 1], F32, "iota")
        kb.op("pool", lambda e: e.iota(IOTA.ap, [[0, 1]], base=0, channel_multiplier=1,
                                       allow_small_or_imprecise_dtypes=True), W=[IOTA])
        kps = Rot([kb.sb([128, 512], F32, f"kp{i}") for i in range(5)])
        vps = Rot([kb.sb([128, 512], F32, f"vp{i}") for i in range(5)])
        prods = Rot([kb.sb([128, 512], F32, f"prod{i}") for i in range(2)])
        import os
        FSL = int(os.environ.get("KDEV_FS", "9"))
        for r in range(NS):
          with kb.scope():
                PTB = kb.sb([128, NPAGE], I32, "ptb")
                kb.dma("sp", PTB, pt[r:r + 1, :].bc([128, NPAGE]))
                PTF = kb.sb([128, NPAGE], F32, "ptf")
                kb.cp("dve", PTF, PTB)
                kb.ts("dve", PTF, PTF, 256.0, float(l * 128), op0=ALU.mult, op1=ALU.add)
                kb.ts("dve", PTF, PTF, IOTA[:, 0:1], None, op0=ALU.add)
                IDX = kb.sb([128, NPAGE], I32, "idx")
                kb.cp("dve", IDX, PTF)
                PTC = kb.sb([128, 1], I32, "ptc")
                kb.dma("sp", PTC, pt[r, :].rr("(p o) -> p o", o=1))
                PCF = kb.sb([128, 1], F32, "pcf")
                kb.cp("dve", PCF, PTC)
                kb.ts("dve", PCF, PCF, 2.0, float(l), op0=ALU.mult, op1=ALU.add)
                IDXQ = kb.sb([128, 1], I32, "idxq")
                kb.cp("dve", IDXQ, PCF)
                LFQ = kb.sb([128, 512], F32, "lfq")
                kb.dma("pool", LFQ, clf, fn=lambda e: e.indirect_dma_start(
                    out=LFQ.ap, out_offset=None, in_=clf.ap,
                    in_offset=bass.IndirectOffsetOnAxis(ap=IDXQ.ap, axis=0)), R=[IDXQ])
                if FSL < 2:
                    continue
                TOTQ = kb.sb([128, 4], F32, "totq")
                kb.red("dve", TOTQ, LFQ.rr("n (p h) -> n h p", h=4))
                psT = psrot.next()
                for h in range(4):
                    kb.tr(psT[:, h * 128:(h + 1) * 128], LFQ.rr("n (p h) -> n h p", h=4)[:, h, :], IDENT)
                LFP = kb.sb([128, 512], F32, "lfp")
                kb.cp("dve", LFP, psT)
                TOTB = kb.sb([128, 4, 128], F32, "totb")
                kb.cp("dve", TOTB, TOTQ.rr("n (h o) -> n h o", o=1).bc([128, 4, 128]))
                psD = psrot.next()
                for h in range(4):
                    kb.mm(psD[:, h * 128:(h + 1) * 128], SUFX, LFP[:, h * 128:(h + 1) * 128], start=True, stop=False)
                    kb.mm(psD[:, h * 128:(h + 1) * 128], TOTB[:, h, :], SUFX, start=False, stop=True)
                psQ = psrot.next()
                kb.mm(psQ, ONESF[0:1, :], QROW[r])
                QB = kb.sb([128, 512], F32, "qb")
                kb.cp("act", QB, psQ)
                psL = psrot.next()
                kb.mm(psL[:, 0:4], ONESF[0:1, :], LF[0:1, 16 + r, :])
                LFNB = kb.sb([128, 4], F32, "lfnb")
                kb.cp("dve", LFNB, psL[:, 0:4])
                if FSL < 4:
                    continue
                SC = kb.sb([128, 4, NPAGE], F32, "sc")
                for n in range(NPAGE):
                    kp = kps.next()
                    kb.dma("pool", kp, ck, fn=lambda e, kp=kp, n=n: e.indirect_dma_start(
                        out=kp.ap, out_offset=None, in_=ck.ap,
                        in_offset=bass.IndirectOffsetOnAxis(ap=IDX[:, n:n + 1].ap, axis=0)), R=[IDX])
                    pr = prods.next()
                    kb.tt("dve", pr, kp, QB, ALU.mult)
                    kb.red("dve", SC[:, :, n], pr.rr("p (h d) -> p h d", h=4))
                if FSL < 5:
                    continue
                SC2 = kb.sb([128, 4, NPAGE], F32, "sc2")
                kb.tt("dve", SC2.rr("p h n -> p (h n)"), SC.rr("p h n -> p (h n)"), psD, ALU.add)
                P = kb.sb([128, 4, NPAGE], F32, "P")
                for h in range(4):
                    kb.act(P[:, h, :], SC2[:, h, :], AF.Exp, bias=LFNB[:, h:h + 1], scale=1.0)
                SP = kb.sb([1, 512], F32, "sprod")
                kb.tt("dve", SP, QROW[r], KNEW[r], ALU.mult)
                WS = kb.sb([1, 4], F32, "ws")
                kb.red("dve", WS, SP.rr("p (h d) -> p h d", h=4))
                kb.act(WS, WS, AF.Exp)
                psS = psrot.next()
                kb.mm(psS[0:1, :], ONESF[:, 0:1], P.rr("p h n -> p (h n)"))
                LS = kb.sb([1, 4], F32, "ls")
                kb.red("dve", LS, psS[0:1, :].rr("p (h n) -> p h n", h=4))
                kb.tt("dve", LS, LS, WS, ALU.add)
                RLS = kb.sb([1, 4], F32, "rls")
                kb.recip(RLS, LS)
                if FSL < 6:
                    continue
                psO = psrot.next()
                for n in range(NPAGE):
                    vp = vps.next()
                    kb.dma("pool", vp, cv, fn=lambda e, vp=vp, n=n: e.indirect_dma_start(
                        out=vp.ap, out_offset=None, in_=cv.ap,
                        in_offset=bass.IndirectOffsetOnAxis(ap=IDX[:, n:n + 1].ap, axis=0)), R=[IDX])
                    kb.mm(psO[0:4, :], P[:, :, n], vp, start=(n == 0), stop=(n == NPAGE - 1))
                if FSL < 7:
                    continue
                OM = kb.sb([4, 512], F32, "om")
                kb.tt("dve", OM, psO[0:4, :], cf("bd", rows=4), ALU.mult)
                psR = psrot.next()
                kb.mm(psR[0:1, :], ONESF[0:4, 0:1], OM)
                YS = kb.sb([1, 512], F32, "ys")
                for h in range(4):
                    hs = slice(h * 128, (h + 1) * 128)
                    kb.stt("dve", YS[:, hs], VNEW[r][:, hs], WS[0:1, h:h + 1], psR[0:1, hs], ALU.mult, ALU.add)
                    kb.ts("dve", YS[:, hs], YS[:, hs], RLS[0:1, h:h + 1], None, op0=ALU.mult)
                psC = psrot.next()
                for h in range(4):
                    kb.mm(psC[:, h:h + 1], YS[0:1, h * 128:(h + 1) * 128], ONESF[0:1, 0:1])
                for h in range(4):
                    kb.cp("dve", YF[h][:, T + r:T + r + 1], psC[:, h:h + 1])


def _mixer_pool(kb, L, g):
    l, W, UT = L["l"], L["W"], L["UT"]
    proj_fm, evac, store_y, psrot = L["proj_fm"], L["evac"], L["store_y"], g["psrot"]
    cf, IDENT = g["cf"], g["IDENT"]
    npool_p, npool_s, spool = g["npool_p"], g["npool_s"], g["spool"]
    NE = 15 + T
    with kb.scope():
        WM = kb.sb([128, 4, 128], BF16, "wmap")
        kb.dma("pool", WM, W["wmap"].rr("g c d -> c g d"))
        PSC = kb.sb([128, 4], F32, "pscale")
        g["load_rows_T"](PSC, W["pscale"], 4)
        NP_P = kb.sb([15, 512], F32, "npoolp")
        NP_S = [kb.sb([1, 512], F32, f"npools{r}") for r in range(NS)]
        HS = [kb.sb([15, 512], F32, f"hist{r}") for r in range(NS)]
        for r in range(NS):
            kb.dma("sp", HS[r], spool[r, l])
            kb.dma("sp", npool_s[r, l, 0:14, :], spool[r, l, 1:15, :])
        RAW = kb.sb([128, NE], F32, "extR")
        EAB = [kb.sb([128, NE], F32, "extA"), kb.sb([128, NE], F32, "extB")]
        XR = [kb.sb([128, 16], F32, f"exts{r}") for r in range(NS)]
        XAB = [[kb.sb([128, 16], F32, f"extsa{r}") for r in range(NS)], [kb.sb([128, 16], F32, f"extsb{r}") for r in range(NS)]]
        DIFF = kb.sb([128, TT], BF16, "diff")
        YP = kb.sb([128, TT], BF16, "yp")
        TMP1 = kb.sb([128, 1], F32, "ptmp")
        kb.memset("pool", RAW[:, 0:15], 0.0)
        for gi, w in enumerate((2, 4, 8, 16)):
            def cons(c0, cn, ps):
                if c0 < T:
                    evac(RAW[:, 15 + c0:15 + c0 + cn], ps[:, 0:cn])
                else:
                    for r in range(NS):
                        kb.cp("dve", XR[r][:, 15:16], ps[:, r:r + 1])
            proj_fm(OFF["pin"] + gi * 128, 128, cons)
            for r in range(NS):
                ps = psrot.next()
                kb.tr(ps[:, 0:15], HS[r][:, gi * 128:(gi + 1) * 128], IDENT[0:15, 0:15])
                kb.cp("dve", XR[r][:, 0:15], ps[:, 0:15])
            ps = psrot.next()
            kb.tr(ps[0:15, 0:128], RAW[:, NE - 15:NE], IDENT)
            kb.cp("dve", NP_P[:, gi * 128:(gi + 1) * 128], ps[0:15, 0:128])
            for r in range(NS):
                ps = psrot.next()
                kb.tr(ps[0:1, 0:128], XR[r][:, 15:16], IDENT)
                kb.cp("dve", NP_S[r][:, gi * 128:(gi + 1) * 128], ps[0:1, 0:128])
            src, ssrc = RAW, XR
            s, i = 1, 0
            while s < w:
                dst, sdst = EAB[i % 2], XAB[i % 2]
                i += 1
                kb.tt("dve", dst[:, s:NE], src[:, s:NE], src[:, 0:NE - s], ALU.add)
                for r in range(NS):
                    kb.tt("pool", sdst[r][:, s:16], ssrc[r][:, s:16], ssrc[r][:, 0:16 - s], ALU.add)
                src, ssrc = dst, sdst
                s *= 2
            sc = EAB[i % 2]
            kb.ts("pool", sc[:, 15:NE], src[:, 15:NE], 1.0 / w, None, op0=ALU.mult)
            kb.tt("pool", sc[:, 15:31], sc[:, 15:31], cf("pfix", lo=gi * 16, hi=(gi + 1) * 16), ALU.mult)
            kb.tt("dve", DIFF[:, 0:T], sc[:, 15:NE], RAW[:, 15:NE], ALU.subtract)
            for r in range(NS):
                kb.ts("dve", TMP1, ssrc[r][:, 15:16], 1.0 / w, None, op0=ALU.mult)
                kb.tt("dve", DIFF[:, T + r:T + r + 1], TMP1, XR[r][:, 15:16], ALU.subtract)
            for (c0, cn) in COLT:
                ps = psrot.next()
                kb.mm(ps[:, 0:cn], WM[:, gi, :], DIFF[:, c0:c0 + cn])
                kb.ts("dve", YP[:, c0:c0 + cn], ps[:, 0:cn], PSC[:, gi:gi + 1], None, op0=ALU.mult)
            store_y(4 + gi, YP)
        kb.dma("sp", npool_p[l], NP_P)
        for r in range(NS):
            kb.dma("sp", npool_s[r, l, 14:15, :], NP_S[r])


def _norm_gate_T(kb, psrot, IDENT, O, m, gate, Ydst, rms, TMP):
    st = TMP["st"]
    if not rms:
        kb.red("dve", st[0:m, 0:1], O[0:m, :])
        kb.ts("dve", st[0:m, 0:1], st[0:m, 0:1], -1.0 / 128.0, None, op0=ALU.mult)
        kb.ts("dve", O[0:m, :], O[0:m, :], st[0:m, 0:1], None, op0=ALU.add)
    kb.memset("dve", st[0:m, 1:2], 0.0)
    kb.act(TMP["sq"][0:m, :], O[0:m, :], AF.Square, accum=st[0:m, 1:2])
    kb.act(st[0:m, 2:3], st[0:m, 1:2], AF.Ln, bias=LN_EPS, scale=1.0 / 128.0)
    kb.act(st[0:m, 2:3], st[0:m, 2:3], AF.Exp, scale=-0.5)
    kb.stt("dve", TMP["y"][0:m, :], O[0:m, :], st[0:m, 2:3], gate, ALU.mult, ALU.mult)
    ps = psrot.next()
    kb.tr(ps[:, 0:m], TMP["y"][0:m, :], IDENT[0:m, 0:m])
    kb.cp("act", Ydst, ps[:, 0:m])


def _mixer_ret(kb, L, g):
    l, W, UT = L["l"], L["W"], L["UT"]
    proj_tm, evac, store_y, psrot = L["proj_tm"], L["evac"], L["store_y"], g["psrot"]
    cf, IDENT, ONESF, PS = g["cf"], g["IDENT"], g["ONESF"], g["PS"]
    nret_p, nret_s, sret, rotc = g["nret_p"], g["nret_s"], g["sret"], g["rotc"]
    GAM, GC = g["GAM"], g["GC"]
    SCALE = 128.0 ** -0.5
    with kb.scope():
        ROT = kb.sb([128, 2, NTILE, 128], F32, "rot")
        kb.dma("sp", ROT.rr("p a t c -> p (a t c)"), rotc)
        RQ = kb.sb([128, NTILE, 128], F32, "rq")
        RK = kb.sb([128, NTILE, 128], F32, "rk")
        RV = kb.sb([128, 16, 128], BF16, "rv")
        SG = kb.sb([128, 16, 128], F32, "sg")
        RVS = [kb.sb([1, 128], F32, f"rvs{r}") for r in range(NS)]
        SGS = [kb.sb([1, 128], F32, f"sgs{r}") for r in range(NS)]
        QRT = kb.sb([128, T], BF16, "qrt")
        KRT = kb.sb([128, T], BF16, "krt")
        KZ = kb.sb([128, 16, 128], BF16, "kz")
        YR = kb.sb([128, TT], BF16, "yr")
        SF = kb.sb([128, 128], F32, "sf")
        SB = kb.sb([128, 128], BF16, "sbf")
        ATT = Rot([kb.sb([128, 128], BF16, f"att{i}") for i in range(2)])
        OT = Rot([kb.sb([128, 128], F32, f"ot{i}") for i in range(2)])
        BX = kb.sb([128, 128], F32, "bx")
        TMP = dict(st=kb.sb([128, 4], F32, "st"), sq=kb.sb([128, 128], F32, "sq"), y=kb.sb([128, 128], F32, "yn"))
        TA = kb.sb([128, 128], F32, "ta")
        TB = kb.sb([128, 128], F32, "tb")
        S0 = kb.sb([128, 128], F32, "s0")
        COL = kb.sb([128, 2], F32, "col")
        SC1 = kb.sb([1, 4], F32, "sc1")
        OS = kb.sb([1, 128], F32, "os")

        def rot_cons(dst):
            def f(ti, m, ps):
                kb.tt("dve", TA[0:m, :], ps[0:m, 0:128], ROT[0:m, 0, ti, :], ALU.mult)
                kb.tt("dve", TB[0:m, 0:64], ps[0:m, 64:128], ROT[0:m, 1, ti, 0:64], ALU.mult)
                kb.tt("dve", TB[0:m, 64:128], ps[0:m, 0:64], ROT[0:m, 1, ti, 64:128], ALU.mult)
                kb.tt("pool", dst[0:m, ti, :], TA[0:m, :], TB[0:m, :], ALU.add)
            return f

        def v_cons(ti, m, ps):
            if ti < 16:
                evac(RV[:, ti, :], ps[:, 0:128])
            else:
                kb.cp("dve", RVS[ti - 16], ps[0:1, 0:128])

        def g_cons(ti, m, ps):
            if ti < 16:
                kb.act(SG[:, ti, :], ps[:, 0:128], AF.Silu)
            else:
                kb.act(SGS[ti - 16], ps[0:1, 0:128], AF.Silu)

        for h in range(4):
            proj_tm(OFF["rq"] + h * 128, 128, rot_cons(RQ))
            proj_tm(OFF["rk"] + h * 128, 128, rot_cons(RK))
            proj_tm(OFF["rv"] + h * 128, 128, v_cons)
            proj_tm(OFF["rg"] + h * 128, 128, g_cons)
            for ti in range(16):
                ps = psrot.next()
                kb.tr(ps[:, 0:128], RQ[:, ti, :], IDENT)
                kb.tr(ps[:, 128:256], RK[:, ti, :], IDENT)
                kb.act(QRT[:, ti * 128:(ti + 1) * 128], ps[:, 0:128], AF.Copy, scale=SCALE)
                kb.cp("dve", KRT[:, ti * 128:(ti + 1) * 128], ps[:, 128:256])
                kb.ts("pool", KZ[:, ti, :], RK[:, ti, :], cf("zeta", lo=h, hi=h + 1), None, op0=ALU.mult)
            kb.memset("dve", SF, 0.0)
            kb.memset("dve", SB, 0.0)
            DM = cf("dmat", lo=h * 128, hi=(h + 1) * 128)
            XI = cf("xi", lo=h, hi=h + 1)
            for n in range(16):
                cs = slice(n * 128, (n + 1) * 128)
                psa = psrot.next()
                kb.mm(psa[:, 0:128], KRT[:, cs], QRT[:, cs])
                at = ATT.next()
                kb.tt("dve", at, psa[:, 0:128], DM, ALU.mult)
                pso = psrot.next()
                kb.mm(pso[:, 0:128], at, RV[:, n, :])
                kb.mm(pso[:, 128:256], QRT[:, cs], SB)
                pss = psrot.next()
                kb.mm(pss[:, 0:128], KZ[:, n, :], RV[:, n, :])
                kb.ts("dve", BX, pso[:, 128:256], XI, None, op0=ALU.mult)
                o = OT.next()
                kb.tt("dve", o, pso[:, 0:128], BX, ALU.add)
                kb.stt("dve", SF, SF, float(GC[h]), pss[:, 0:128], ALU.mult, ALU.add)
                kb.cp("act", SB, SF)
                _norm_gate_T(kb, psrot, IDENT, o, 128, SG[:, n, :], YR[:, cs], False, TMP)
            kb.dma("sp", nret_p[l, h], SF)
            for r in range(NS):
                ti = 16 + r
                kb.dma("sp", S0, sret[r, l, h])
                ps = psrot.next()
                kb.mm(ps[:, 0:1], RQ[0:1, ti, :], ONESF[0:1, 0:1])
                kb.ts("dve", COL[:, 0:1], ps[:, 0:1], SCALE, None, op0=ALU.mult)
                ps2 = psrot.next()
                kb.mm(ps2[0:1, 0:128], COL[:, 0:1], S0)
                kb.tt("dve", TA[0:1, :], RQ[0:1, ti, :], RK[0:1, ti, :], ALU.mult)
                kb.red("dve", SC1[:, 0:1], TA[0:1, :])
                kb.ts("dve", SC1[:, 0:1], SC1[:, 0:1], SCALE, None, op0=ALU.mult)
                kb.ts("dve", OS, ps2[0:1, 0:128], float(GAM[h]), None, op0=ALU.mult)
                kb.stt("dve", OS, RVS[r], SC1[0:1, 0:1], OS, ALU.mult, ALU.add)
                ps3 = psrot.next()
                kb.mm(ps3[:, 0:128], RK[0:1, ti, :], RVS[r])
                kb.stt("dve", TB, S0, float(GAM[h]), ps3[:, 0:128], ALU.mult, ALU.add)
                kb.dma("sp", nret_s[r, l, h], TB)
                _norm_gate_T(kb, psrot, IDENT, OS, 1, SGS[r], YR[:, T + r:T + r + 1], False, TMP)
            store_y(8 + h, YR)


def _mixer_gla(kb, L, g):
    l, W, UT = L["l"], L["W"], L["UT"]
    proj_tm, proj_fm, evac, store_y, psrot = L["proj_tm"], L["proj_fm"], L["evac"], L["store_y"], g["psrot"]
    cf, IDENT, ONESF, PS = g["cf"], g["IDENT"], g["ONESF"], g["PS"]
    ngla_p, ngla_s, sgla = g["ngla_p"], g["ngla_s"], g["sgla"]
    TRI64, SEL64 = cf("tri64"), cf("sel64")
    SCALE = 64.0 ** -0.5
    with kb.scope():
        GLRT = kb.sb([16, TT], BF16, "glrt")
        WA2 = kb.sb([16, 256], BF16, "wa2")
        kb.dma("pool", WA2, W["wa2"])
        BA = kb.sb([128, 256], F32, "ba")
        kb.dma("sp", BA, W["ba"].bc([128, 256]))
        proj_fm(OFF["glr"], 16, lambda c0, cn, ps: evac(GLRT[:, c0:c0 + cn], ps[0:16, 0:cn]))
        Bc = kb.sb([128, NTILE, 256], F32, "Bc")
        BLc = kb.sb([128, 16, 256], F32, "BLc")
        LG = Rot([kb.sb([128, 256], F32, f"lg{i}") for i in range(2)])
        for ti in range(NTILE):
            c0, m = L["tok_range"](ti)
            ps = psrot.next()
            kb.mm(ps[0:m, 0:256], GLRT[:, c0:c0 + m], WA2)
            lg = LG.next()
            kb.tt("dve", lg[0:m, :], ps[0:m, 0:256], BA[0:m, :], ALU.add)
            kb.act(lg[0:m, :], lg[0:m, :], AF.Exp, scale=-1.0)
            kb.act(lg[0:m, :], lg[0:m, :], AF.Ln, bias=1.0)
            kb.ts("dve", lg[0:m, :], lg[0:m, :], -1.0 / 16.0, None, op0=ALU.mult)
            if ti < 16:
                ps2 = psrot.next()
                kb.mm(ps2[:, 0:256], TRI64, lg)
                kb.cp("act", Bc[:, ti, :], ps2[:, 0:256])
                ps3 = psrot.next()
                kb.mm(ps3[:, 0:256], SEL64, Bc[:, ti, :])
                kb.cp("dve", BLc[:, ti, :], ps3[:, 0:256])
            else:
                kb.cp("dve", Bc[0:1, ti, :], lg[0:1, :])
        GQ = kb.sb([128, NTILE, 64], F32, "gq")
        GK = kb.sb([128, NTILE, 64], F32, "gk")
        GV = kb.sb([128, 16, 128], BF16, "gv")
        SGG = kb.sb([128, 16, 128], F32, "sgg")
        GVS = [kb.sb([1, 128], F32, f"gvs{r}") for r in range(NS)]
        SGS = [kb.sb([1, 128], F32, f"gsgs{r}") for r in range(NS)]
        YG = kb.sb([128, TT], BF16, "yg")
        SF = kb.sb([64, 128], F32, "gsf")
        SBs = Rot([kb.sb([64, 128], BF16, f"gsb{i}") for i in range(3)])
        EB = kb.sb([128, 64], F32, "eb")
        ENB = kb.sb([128, 64], F32, "enb")
        EZ = kb.sb([128, 64], F32, "ez")
        QB = kb.sb([128, 64], F32, "qbb")
        KBt = kb.sb([128, 64], F32, "kbb")
        KZA = kb.sb([128, 64], BF16, "gkza")
        KZB = kb.sb([128, 64], BF16, "gkzb")
        QBT = kb.sb([64, 128], BF16, "qbt")
        QBTA = kb.sb([64, 128], BF16, "qbta")
        QBTB = kb.sb([64, 128], BF16, "qbtb")
        kb.memset("dve", QBTA, 0.0)
        kb.memset("dve", QBTB, 0.0)
        MA = cf("tri64", lo=63, hi=64)
        MB = cf("tri64", lo=127, hi=128)
        KBT = kb.sb([64, 128], BF16, "kbt")
        EBL = kb.sb([64, 2], F32, "ebl")
        ATT = kb.sb([128, 128], BF16, "gatt")
        O = kb.sb([128, 128], F32, "go")
        TMP = dict(st=kb.sb([128, 4], F32, "gst"), sq=kb.sb([128, 128], F32, "gsq"), y=kb.sb([128, 128], F32, "gyn"))
        S0 = kb.sb([64, 128], F32, "gs0")
        SN = kb.sb([64, 128], F32, "gsn")
        COL = kb.sb([64, 2], F32, "gcol")
        SC1 = kb.sb([1, 4], F32, "gsc1")
        OS = kb.sb([1, 128], F32, "gos")
        R1 = kb.sb([1, 64], F32, "gr1")
        R2 = kb.sb([1, 64], F32, "gr2")

        def v_cons(ti, m, ps):
            if ti < 16:
                evac(GV[:, ti, :], ps[:, 0:128])
            else:
                kb.cp("dve", GVS[ti - 16], ps[0:1, 0:128])

        def g_cons(ti, m, ps):
            if ti < 16:
                kb.act(SGG[:, ti, :], ps[:, 0:128], AF.Silu)
            else:
                kb.act(SGS[ti - 16], ps[0:1, 0:128], AF.Silu)

        for h in range(4):
            hs = slice(h * 64, (h + 1) * 64)
            proj_tm(OFF["gq"] + h * 64, 64, lambda ti, m, ps: evac(GQ[0:m, ti, :], ps[0:m, 0:64], scale=SCALE))
            proj_tm(OFF["gk"] + h * 64, 64, lambda ti, m, ps: evac(GK[0:m, ti, :], ps[0:m, 0:64]))
            proj_tm(OFF["gv"] + h * 128, 128, v_cons)
            proj_tm(OFF["gg"] + h * 128, 128, g_cons)
            kb.memset("dve", SF, 0.0)
            sb_cur = SBs.next()
            kb.memset("dve", sb_cur, 0.0)
            for ti in range(16):
                cs = slice(ti * 128, (ti + 1) * 128)
                kb.act(EB, Bc[:, ti, hs], AF.Exp)
                kb.act(ENB, Bc[:, ti, hs], AF.Exp, scale=-1.0)
                kb.tt("dve", EZ, BLc[:, ti, hs], Bc[:, ti, hs], ALU.subtract)
                kb.act(EZ, EZ, AF.Exp)
                kb.tt("dve", QB, GQ[:, ti, :], EB, ALU.mult)
                kb.tt("pool", KBt, GK[:, ti, :], ENB, ALU.mult)
                kb.stt("dve", KZA, GK[:, ti, :], MA, EZ, ALU.mult, ALU.mult)
                kb.stt("dve", KZB, GK[:, ti, :], MB, EZ, ALU.mult, ALU.mult)
                ps = psrot.next()
                kb.tr(ps[0:64, 0:128], QB, IDENT)
                kb.tr(ps[0:64, 128:256], KBt, IDENT)
                kb.tr(ps[0:64, 256:384], BLc[:, ti, hs], IDENT)
                kb.cp("dve", QBT, ps[0:64, 0:128])
                kb.cp("dve", QBTA[:, 0:64], ps[0:64, 0:64])
                kb.cp("dve", QBTB[:, 64:128], ps[0:64, 64:128])
                kb.cp("act", KBT, ps[0:64, 128:256])
                kb.act(EBL[:, 0:1], ps[0:64, 256:257], AF.Exp)
                kb.act(EBL[:, 1:2], ps[0:64, 320:321], AF.Exp)
                psa = psrot.next()
                kb.mm(psa[:, 0:128], KBT, QBT)
                kb.tt("dve", ATT, psa[:, 0:128], TRI64, ALU.mult)
                pss = psrot.next()
                kb.mm(pss[0:64, 0:128], KZA, GV[:, ti, :])
                kb.stt("dve", SF, SF, EBL[:, 0:1], pss[0:64, 0:128], ALU.mult, ALU.add)
                sb_mid = SBs.next()
                kb.cp("act", sb_mid, SF)
                pso = psrot.next()
                kb.mm(pso[:, 0:128], ATT, GV[:, ti, :], start=True, stop=False)
                kb.mm(pso[:, 0:128], QBTA, sb_cur, start=False, stop=False)
                kb.mm(pso[:, 0:128], QBTB, sb_mid, start=False, stop=True)
                kb.cp("dve", O, pso[:, 0:128])
                pss2 = psrot.next()
                kb.mm(pss2[0:64, 0:128], KZB, GV[:, ti, :])
                kb.stt("dve", SF, SF, EBL[:, 1:2], pss2[0:64, 0:128], ALU.mult, ALU.add)
                sb_cur = SBs.next()
                kb.cp("act", sb_cur, SF)
                _norm_gate_T(kb, psrot, IDENT, O, 128, SGG[:, ti, :], YG[:, cs], True, TMP)
            kb.dma("sp", ngla_p[l, h], SF)
            for r in range(NS):
                ti = 16 + r
                kb.dma("sp", S0, sgla[r, l, h])
                kb.act(R1, Bc[0:1, ti, hs], AF.Exp)
                kb.tt("dve", R2, GQ[0:1, ti, :], R1, ALU.mult)
                ps = psrot.next()
                kb.mm(ps[0:64, 0:1], R2, ONESF[0:1, 0:1])
                kb.mm(ps[0:64, 1:2], R1, ONESF[0:1, 0:1])
                kb.cp("dve", COL, ps[0:64, 0:2])
                ps2 = psrot.next()
                kb.mm(ps2[0:1, 0:128], COL[:, 0:1], S0)
                kb.tt("dve", R2, GQ[0:1, ti, :], GK[0:1, ti, :], ALU.mult)
                kb.red("dve", SC1[:, 0:1], R2)
                kb.stt("dve", OS, GVS[r], SC1[0:1, 0:1], ps2[0:1, 0:128], ALU.mult, ALU.add)
                ps3 = psrot.next()
                kb.mm(ps3[0:64, 0:128], GK[0:1, ti, :], GVS[r])
                kb.stt("dve", SN, S0, COL[:, 1:2], ps3[0:64, 0:128], ALU.mult, ALU.add)
                kb.dma("sp", ngla_s[r, l, h], SN)
                _norm_gate_T(kb, psrot, IDENT, OS, 1, SGS[r], YG[:, T + r:T + r + 1], True, TMP)
            store_y(12 + h, YG)


def _phase_merge(kb, L, g):
    l, W, UT, winv = L["l"], L["W"], L["UT"], L["winv"]
    psrot, ytS, mgS = g["psrot"], g["ytS"], g["mgS"]
    slab_pool = L["slab_pool"]
    wbrv = W["wbr"].rr("n (cc p) d -> p (n cc) d", p=128)
    with kb.scope():
        YB = Rot([kb.sb([128, 16, BW], BF16, f"yb{i}") for i in range(1)])
        MBt = Rot([kb.sb([128, KC, BW], BF16, f"mbt{i}") for i in range(2)])
        BSL = Rot([kb.sb([128, 16, 256], BF16, f"bsl{i}") for i in range(2)])
        gpool = Rot([kb.sb([128, KC, 1024], BF16, f"gslab{i}") for i in range(2)])
        SGt = Rot([kb.sb([128, BW], F32, f"sgt{i}") for i in range(2)])
        TM = Rot([kb.sb([128, BW], F32, f"tmt{i}") for i in range(2)])
        ACC = Rot([kb.sb([128, BW], F32, f"acc{i}") for i in range(2)])
        for bi in range(NB):
            b0 = bi * BW
            yb = YB.next()
            kb.dma("sp", yb, ytS[:, :, b0:b0 + BW].rr("c p t -> p c t"))
            mb = MBt.next()
            for j2 in range(KC // 2):
                gs = gpool.next()
                for n in range(4):
                    c0 = OFF["gates"] + n * D + j2 * 256
                    kb.dma("pool", gs[:, :, n * 256:(n + 1) * 256], winv[:, :, c0:c0 + 256])
                bs = BSL.next()
                kb.dma("pool", bs, wbrv[:, :, j2 * 256:(j2 + 1) * 256])
                for ji in range(2):
                    j = j2 * 2 + ji
                    acc = ACC.next()
                    for n in range(4):
                        psg = psrot.next()
                        for k in range(KC):
                            kb.mm(psg[:, 0:BW], gs[:, k, n * 256 + ji * 128:n * 256 + (ji + 1) * 128], UT[:, k, b0:b0 + BW],
                                  start=(k == 0), stop=(k == KC - 1))
                        psy = psrot.next()
                        for cc in range(4):
                            kb.mm(psy[:, 0:BW], bs[:, n * 4 + cc, ji * 128:(ji + 1) * 128], yb[:, n * 4 + cc, :],
                                  start=(cc == 0), stop=(cc == 3))
                        sg = SGt.next()
                        kb.act(sg, psg[:, 0:BW], AF.Sigmoid)
                        if n == 0:
                            kb.tt("dve", acc, sg, psy[:, 0:BW], ALU.mult)
                        else:
                            tm = TM.next()
                            kb.tt("dve", tm, sg, psy[:, 0:BW], ALU.mult)
                            kb.tt("dve", acc, acc, tm, ALU.add)
                    kb.cp("act", mb[:, j, :], acc)
            kb.dma("sp", mgS[bi], mb)


def _segs(bi):
    b0 = bi * BW
    out = []
    a = 0
    while a < BW:
        gcol = b0 + a
        if gcol < T:
            b = min(BW, T - b0)
            out.append((a, b, 0))
            a = b
        else:
            out.append((a, a + 1, 1 + gcol - T))
            a += 1
    return out


def _phase_ffn(kb, L, g):
    l, W, last = L["l"], L["W"], L["last"]
    psrot, mgS, xaT, cf = g["psrot"], g["mgS"], g["xaT"], g["cf"]
    MOD, LNP, IDENT, ONESF = g["MOD"][l], g["LNP"][l], g["IDENT"], g["ONESF"]
    y_p, y_s = g["y_p"], g["y_s"]
    moe = (l % 2 == 1)
    wov = W["wo"].rr("(k p) n -> p k n", p=128)
    with kb.scope():
        AG1 = kb.sb([128, KC], F32, "ag1")
        AB1 = kb.sb([128, KC], F32, "ab1")
        kb.ts("dve", AG1, LNP[:, 0], ALPHA, None, op0=ALU.mult)
        kb.ts("dve", AB1, LNP[:, 1], ALPHA, None, op0=ALU.mult)
        HG = kb.sb([128, KC, 3], F32, "hg")
        HB = kb.sb([128, KC, 3], F32, "hb")
        for r in range(3):
            kb.tt("dve", HG[:, :, r], LNP[:, 0], MOD[:, 4, :, r], ALU.mult)
            kb.tt("dve", HB[:, :, r], LNP[:, 1], MOD[:, 4, :, r], ALU.mult)
            kb.tt("dve", HB[:, :, r], HB[:, :, r], MOD[:, 3, :, r], ALU.add)
        OG = kb.sb([128, KC], F32, "og")
        OB = kb.sb([128, KC], F32, "ob")
        kb.ts("dve", OG, LNP[:, 2], 1.0 if last else ALPHA, None, op0=ALU.mult)
        kb.ts("dve", OB, LNP[:, 3], 1.0 if last else ALPHA, None, op0=ALU.mult)
        MB = kb.sb([128, KC, BW], BF16, "mb")
        Y1 = kb.sb([128, KC, BW], F32, "y1")
        HV = MB
        C = kb.sb([128, KC, BW], F32, "cacc")
        HT = Rot([kb.sb([128, FCH, BW], BF16, f"ht{i}") for i in range(1)])
        GUS = Rot([kb.sb([128, KC, 512], BF16, f"gus{i}") for i in range(2)])
        DNS = Rot([kb.sb([128, FCH, 256], BF16, f"dns{i}") for i in range(2)])
        WOS = Rot([kb.sb([128, KC, 128], BF16, f"wos{i}") for i in range(2)])
        XA = Rot([kb.sb([128, BW], F32, f"xab{i}") for i in range(2)])
        SQ = Rot([kb.sb([128, BW], F32, f"sq{i}") for i in range(2)])
        HVF = Rot([kb.sb([128, BW], F32, f"hvf{i}") for i in range(1)])
        SGt = Rot([kb.sb([128, BW], F32, f"fsg{i}") for i in range(2)])
        TMt = Rot([kb.sb([128, BW], F32, f"ftm{i}") for i in range(1)])
        MEAN = kb.sb([128, BW], F32, "mean")
        M2 = kb.sb([128, BW], F32, "m2")
        RSTD = kb.sb([128, BW], F32, "rstd")
        if moe:
            WRF = kb.sb([128, KC, 8], F32, "wrf")
            kb.dma("sp", WRF, W["wr"].rr("(k p) x -> p k x", p=128))
            BR = kb.sb([8, 1], F32, "br")
            kb.dma("sp", BR, W["br"])
            LOG = kb.sb([8, BW], F32, "log")
            LT = kb.sb([128, 4, 8], F32, "lt")
            GT = kb.sb([128, 4, 8], F32, "gt")
            GTF = kb.sb([8, BW], F32, "gtf")
            GB = kb.sb([128, NEXP, BW], F32, "gb")
            TK = kb.sb([128, 8, 8], F32, "tk")
            EQb = kb.sb([128, 4, 8], F32, "eqb")
            L2b = kb.sb([128, 4, 8], F32, "l2b")
            SELb = kb.sb([128, 4, 8], F32, "selb")
            EXb = kb.sb([128, 4, 8], F32, "exb")
            kb.memset("dve", LT, 0.0)
        if last:
            YTM = Rot([kb.sb([128, D], F32, f"ytm{i}") for i in range(1)])
        SUBS = [(0, 128), (128, 128), (256, 128), (384, BW - 384)]

        def layer_norm(Y):
            ps1 = psrot.next()
            ps2 = psrot.next()
            for e in range(KC):
                kb.mm(ps1[:, 0:BW], ONESF, Y[:, e, :], start=(e == 0), stop=(e == KC - 1))
                sq = SQ.next()
                kb.act(sq, Y[:, e, :], AF.Square)
                kb.mm(ps2[:, 0:BW], ONESF, sq, start=(e == 0), stop=(e == KC - 1))
            kb.ts("dve", MEAN, ps1[:, 0:BW], 1.0 / D, None, op0=ALU.mult)
            kb.tt("dve", M2, MEAN, MEAN, ALU.mult)
            kb.stt("dve", RSTD, ps2[:, 0:BW], 1.0 / D, M2, ALU.mult, ALU.subtract)
            kb.act(RSTD, RSTD, AF.Ln, bias=LN_EPS)
            kb.act(RSTD, RSTD, AF.Exp, scale=-0.5)
            for e in range(KC):
                eng = "dve" if e % 2 == 0 else "pool"
                kb.tt(eng, Y[:, e, :], Y[:, e, :], MEAN, ALU.subtract)
                kb.tt(eng, Y[:, e, :], Y[:, e, :], RSTD, ALU.mult)

        import os
        FFL = int(os.environ.get("KDEV_FFN", "9"))
        for bi in range(NB):
            b0 = bi * BW
            segs = _segs(bi)
            kb.dma("sp", MB, mgS[bi])
            for e in range(KC):
                ws = WOS.next()
                kb.dma("pool", ws, wov[:, :, e * 128:(e + 1) * 128])
                ps = psrot.next()
                for k in range(KC):
                    kb.mm(ps[:, 0:BW], ws[:, k, :], MB[:, k, :], start=(k == 0), stop=(k == KC - 1))
                xa = XA.next()
                kb.dma("sp", xa, xaT[e, :, b0:b0 + BW])
                for (a, b, r) in segs:
                    kb.stt("dve", Y1[:, e, a:b], ps[:, a:b], MOD[:, 2, e, r:r + 1], xa[:, a:b], ALU.mult, ALU.add)
            if FFL < 2:
                continue
            layer_norm(Y1)
            if FFL < 3:
                continue
            if moe:
                psr = psrot.next()
            for e in range(KC):
                hvf = HVF.next()
                for (a, b, r) in segs:
                    kb.ts("dve", hvf[:, a:b], Y1[:, e, a:b], HG[:, e, r:r + 1], HB[:, e, r:r + 1], op0=ALU.mult, op1=ALU.add)
                if moe:
                    kb.mm(psr[0:8, 0:BW], WRF[:, e, :], hvf, start=(e == 0), stop=(e == KC - 1))
                kb.cp("act", HV[:, e, :], hvf)
                kb.ts("dve", Y1[:, e, :], Y1[:, e, :], AG1[:, e:e + 1], AB1[:, e:e + 1], op0=ALU.mult, op1=ALU.add)
            if moe:
                kb.ts("dve", LOG, psr[0:8, 0:BW], BR[:, 0:1], None, op0=ALU.add)
                pst = psrot.next()
                for si, (s0, sn) in enumerate(SUBS):
                    kb.tr(pst[0:sn, si * 8:(si + 1) * 8], LOG[:, s0:s0 + sn], IDENT[0:8, 0:8])
                for si, (s0, sn) in enumerate(SUBS):
                    kb.cp("dve", LT[0:sn, si, :], pst[0:sn, si * 8:(si + 1) * 8])
                m1, m2, t0, t1 = TK[:, 0, 0:4], TK[:, 1, 0:4], TK[:, 2, 0:4], TK[:, 3, 0:4]
                EQ, L2, SEL, EX = TK[:, 4:8, :], GT, TK[:, 4:8, :], GT
                kb.red("dve", m1, LT, op=ALU.max)
                for si in range(4):
                    kb.ts("dve", EQb[:, si, :], LT[:, si, :], m1[:, si:si + 1], None, op0=ALU.is_equal)
                kb.stt("dve", L2b, EQb, -1e30, LT, ALU.mult, ALU.add)
                kb.red("dve", m2, L2b, op=ALU.max)
                kb.tt("dve", t0, m2, m1, ALU.subtract)
                kb.act(t0, t0, AF.Exp)
                kb.ts("dve", t0, t0, 1.0, None, op0=ALU.add)
                kb.recip(t0, t0)
                kb.ts("dve", t1, m1, -1.0, None, op0=ALU.mult)
                for si in range(4):
                    kb.ts("dve", SELb[:, si, :], LT[:, si, :], m2[:, si:si + 1], None, op0=ALU.is_ge)
                    kb.act(EXb[:, si, :], LT[:, si, :], AF.Exp, bias=t1[:, si:si + 1], scale=1.0)
                    kb.stt("dve", GT[:, si, :], EXb[:, si, :], t0[:, si:si + 1], SELb[:, si, :], ALU.mult, ALU.mult)
                psg = psrot.next()
                for si, (s0, sn) in enumerate(SUBS):
                    kb.tr(psg[0:8, s0:s0 + sn], GT[0:sn, si, :], IDENT[0:sn, 0:sn])
                kb.cp("dve", GTF, psg[0:8, 0:BW])
                for x in range(NEXP):
                    psb = psrot.next()
                    kb.mm(psb[:, 0:BW], cf("sel8", rows=8, lo=x * 128, hi=(x + 1) * 128), GTF)
                    kb.cp("act", GB[:, x, :], psb[:, 0:BW])
            if FFL < 4:
                continue
            nx = NEXP if moe else 2
            for x in range(nx):
                if moe:
                    wg = W["eg"][x].rr("(k p) n -> p k n", p=128)
                    wu = W["eu"][x].rr("(k p) n -> p k n", p=128)
                    wd = W["ed"][x].rr("(f p) n -> p f n", p=128)
                else:
                    wg = W["fg"][:, x * DFE:(x + 1) * DFE].rr("(k p) n -> p k n", p=128)
                    wu = W["fu"][:, x * DFE:(x + 1) * DFE].rr("(k p) n -> p k n", p=128)
                    wd = W["fd"][x * DFE:(x + 1) * DFE, :].rr("(f p) n -> p f n", p=128)
                ht = HT.next()
                for f2 in range(FCH // 2):
                    gu = GUS.next()
                    kb.dma("pool", gu[:, :, 0:256], wg[:, :, f2 * 256:(f2 + 1) * 256])
                    kb.dma("pool", gu[:, :, 256:512], wu[:, :, f2 * 256:(f2 + 1) * 256])
                    for fi in range(2):
                        f = f2 * 2 + fi
                        psg = psrot.next()
                        psu = psrot.next()
                        for k in range(KC):
                            kb.mm(psg[:, 0:BW], gu[:, k, fi * 128:(fi + 1) * 128], HV[:, k, :], start=(k == 0), stop=(k == KC - 1))
                        for k in range(KC):
                            kb.mm(psu[:, 0:BW], gu[:, k, 256 + fi * 128:256 + (fi + 1) * 128], HV[:, k, :], start=(k == 0), stop=(k == KC - 1))
                        sg = SGt.next()
                        kb.act(sg, psg[:, 0:BW], AF.Silu)
                        if moe:
                            tm = TMt.next()
                            kb.tt("dve", tm, sg, psu[:, 0:BW], ALU.mult)
                            kb.tt("dve", ht[:, f, :], tm, GB[:, x, :], ALU.mult)
                        else:
                            kb.tt("dve", ht[:, f, :], sg, psu[:, 0:BW], ALU.mult)
                for e2 in range(KC // 2):
                    ds = DNS.next()
                    kb.dma("pool", ds, wd[:, :, e2 * 256:(e2 + 1) * 256])
                    for ei in range(2):
                        e = e2 * 2 + ei
                        psd = psrot.next()
                        for f in range(FCH):
                            kb.mm(psd[:, 0:BW], ds[:, f, ei * 128:(ei + 1) * 128], ht[:, f, :], start=(f == 0), stop=(f == FCH - 1))
                        if x == 0:
                            kb.cp("act", C[:, e, :], psd[:, 0:BW])
                        else:
                            kb.tt("dve", C[:, e, :], C[:, e, :], psd[:, 0:BW], ALU.add)
            if FFL < 5:
                continue
            for e in range(KC):
                for (a, b, r) in segs:
                    kb.stt("dve", C[:, e, a:b], C[:, e, a:b], MOD[:, 5, e, r:r + 1], Y1[:, e, a:b], ALU.mult, ALU.add)
            layer_norm(C)
            for e in range(KC):
                kb.ts("dve" if e % 2 == 0 else "pool", C[:, e, :], C[:, e, :], OG[:, e:e + 1], OB[:, e:e + 1],
                      op0=ALU.mult, op1=ALU.add)
            if FFL < 6:
                continue
            if not last:
                kb.dma("sp", xaT[:, :, b0:b0 + BW].rr("k p c -> p k c"), C, acc=True)
            else:
                for (s0, sn) in SUBS:
                    ytm = YTM.next()
                    for q4 in range(4):
                        ps = psrot.next()
                        for qq in range(4):
                            e = q4 * 4 + qq
                            kb.tr(ps[0:sn, qq * 128:(qq + 1) * 128], C[:, e, s0:s0 + sn], IDENT)
                        kb.cp("act" if q4 % 2 else "dve", ytm[0:sn, q4 * 512:(q4 + 1) * 512], ps[0:sn, :])
                    g0 = b0 + s0
                    npr = max(0, min(sn, T - g0))
                    if npr > 0:
                        kb.dma("sp", y_p[g0:g0 + npr, :], ytm[0:npr, :])
                    if npr < sn:
                        r0 = g0 + npr - T
                        kb.dma("sp", y_s[r0:r0 + (sn - npr), :], ytm[npr:sn, :])


_CACHE = {}


def core_inputs(z, c, wts, consts):
    cf_, rot_ = consts
    n_pool = z["cache_k"].shape[0]
    inp = dict(wts)
    inp["xp"] = z["x_prompt"][c]
    inp["xs"] = z["x_sample"][NS * c:NS * c + NS, 0, :]
    inp["crows"] = np.concatenate([z["c_prompt"][c:c + 1], z["c_sample"][NS * c:NS * c + NS]], 0)
    inp["ck"] = z["cache_k"].reshape(n_pool * 2 * 128, 512)
    inp["cv"] = z["cache_v"].reshape(n_pool * 2 * 128, 512)
    inp["clf"] = z["cache_logf"].reshape(n_pool * 2, 512)
    inp["pt"] = z["page_table"][NS * c:NS * c + NS].astype(np.int32)
    inp["spool"] = z["state_pool"][NS * c:NS * c + NS]
    inp["sret"] = z["state_ret"][NS * c:NS * c + NS]
    inp["sgla"] = z["state_gla"][NS * c:NS * c + NS]
    inp["constf"] = cf_
    inp["rotc"] = rot_
    return {k: np.ascontiguousarray(v) for k, v in inp.items()}


def kernel(**inputs):
    z = {k: np.asarray(v) for k, v in inputs.items()}
    if "nc" not in _CACHE:
        _CACHE["nc"] = build_program()
    nc = _CACHE["nc"]
    cf_, rot_, _, _ = make_consts()
    wts = weight_inputs(z)
    in_maps = [core_inputs(z, c, wts, (cf_, rot_)) for c in range(NCORE)]
    res = run_bass_kernel_spmd(nc, in_maps, core_ids=list(range(NCORE)))
    R = res.results
    B, SB = NCORE, NCORE * NS

    def cat(name):
        return np.stack([R[c][name] for c in range(NCORE)], 0)

    y_prompt = cat("y_p")
    y_sample = cat("y_s").reshape(SB, 1, D)
    nk_p = cat("nk_p").reshape(B, DEPTH, T, 4, 128)
    nv_p = cat("nv_p").reshape(B, DEPTH, T, 4, 128)
    nlf_p = cat("nlf_p")
    npool_p = cat("npool_p")
    nret_p = cat("nret_p")
    ngla_p = cat("ngla_p")
    nk_s = cat("nk_s").reshape(SB, DEPTH, 1, 4, 128)
    nv_s = cat("nv_s").reshape(SB, DEPTH, 1, 4, 128)
    nlf_s = cat("nlf_s").reshape(SB, DEPTH, 1, 4)
    npool_s = cat("npool_s").reshape(SB, DEPTH, 15, 512)
    nret_s = cat("nret_s").reshape(SB, DEPTH, 4, 128, 128)
    ngla_s = cat("ngla_s").reshape(SB, DEPTH, 4, 64, 128)
    return tuple(np.ascontiguousarray(a.astype(np.float32, copy=False)) for a in
                 (y_prompt, y_sample, nk_p, nv_p, nlf_p, npool_p, nret_p, ngla_p,
                  nk_s, nv_s, nlf_s, npool_s, nret_s, ngla_s))
```
